# Optimizing a Trainium2 kernel written in Bass

```python
import math
import jax, jax.numpy as jnp
from jax import lax
import numpy as np

D_MODEL = 1024
BATCH = 8
SEQ = 2048
DEPTH = 2

ATTN_PATTERNS = ((128, 1), (512, 4), (2048, 16))
N_ATTN_GROUPS = len(ATTN_PATTERNS)
ATTN_HEADS = 8
ATTN_HEAD_DIM = 64
ATTN_WIDTH = ATTN_HEADS * ATTN_HEAD_DIM
RET_HEADS = 4
RET_HEAD_DIM = D_MODEL // RET_HEADS
RET_WIDTH = RET_HEADS * RET_HEAD_DIM
RET_CHUNK = 128
N_EXPERTS = 16
EXPERT_FF = 2 * D_MODEL
CAPACITY_FACTOR = 2
EPS = 1e-6
ATTN_IN = N_ATTN_GROUPS * 3 * ATTN_WIDTH
RET_IN = 4 * RET_WIDTH
GATE_IN = 2 * D_MODEL
N_IN = ATTN_IN + RET_IN + GATE_IN
SPLITS = (ATTN_IN,
          ATTN_IN + RET_WIDTH,
          ATTN_IN + 2 * RET_WIDTH,
          ATTN_IN + 3 * RET_WIDTH,
          ATTN_IN + 4 * RET_WIDTH)

kernel_name = "hybrid_dilated_attn_retention_ec_moe"


def rms_norm(x, gain):
    xf = x.astype(jnp.float32)
    y = xf * lax.rsqrt(jnp.mean(xf * xf, axis=-1, keepdims=True) + EPS)
    return (y * gain.astype(jnp.float32)).astype(x.dtype)


def alibi_slopes(n):
    return 2.0 ** (-8.0 * (jnp.arange(n, dtype=jnp.float32) + 1.0) / n)


def dilated_band_attention(q, k, v, dilation, half, slopes):
    B, S, H, Dh = q.shape
    L = S // dilation
    nb = -(-L // half)
    Lp = nb * half

    def to_residue(t):
        return t.reshape(B, L, dilation, H, Dh).transpose(0, 3, 2, 1, 4)

    qr = jnp.pad(to_residue(q), ((0, 0), (0, 0), (0, 0), (0, Lp - L), (0, 0)))
    qr = qr.reshape(B, H, dilation, nb, half, Dh)

    def windows(t):
        t = jnp.pad(to_residue(t), ((0, 0), (0, 0), (0, 0), (half, Lp - L + half), (0, 0)))
        t = t.reshape(B, H, dilation, nb + 2, half, Dh)
        return jnp.concatenate([t[:, :, :, :-2], t[:, :, :, 1:-1], t[:, :, :, 2:]], axis=4)

    kw, vw = windows(k), windows(v)
    qi = jnp.arange(half)
    ki = jnp.arange(3 * half)
    rel = ki[None, :] - half - qi[:, None]
    kpos = jnp.arange(nb)[:, None] * half - half + ki[None, :]
    valid = (jnp.abs(rel) <= half)[None] & ((kpos >= 0) & (kpos < L))[:, None, :]
    dist = (dilation * jnp.abs(rel)).astype(jnp.float32)

    s = jnp.einsum('bhrnqd,bhrnkd->bhrnqk', qr, kw).astype(jnp.float32) * (Dh ** -0.5)
    s = s - slopes[None, :, None, None, None, None] * dist
    s = jnp.where(valid, s, -jnp.inf)
    m = jnp.max(s, axis=-1, keepdims=True)
    p = jnp.exp(s - m)
    den = jnp.sum(p, axis=-1)
    o = jnp.einsum('bhrnqk,bhrnkd->bhrnqd', p, vw.astype(jnp.float32)) / den[..., None]
    lse = m[..., 0] + jnp.log(den)

    o = o.reshape(B, H, dilation, Lp, Dh)[:, :, :, :L]
    o = o.transpose(0, 3, 2, 1, 4).reshape(B, S, H, Dh)
    lse = lse.reshape(B, H, dilation, Lp)[:, :, :, :L]
    lse = lse.transpose(0, 3, 2, 1).reshape(B, S, H)
    return o, lse


def retention_chunkwise(q, k, v, log_gamma, strict):
    B, H, S, Dk = q.shape
    Dv = v.shape[-1]
    N = S // RET_CHUNK
    qc = q.reshape(B, H, N, RET_CHUNK, Dk).astype(jnp.float32) * (Dk ** -0.5)
    kc = k.reshape(B, H, N, RET_CHUNK, Dk).astype(jnp.float32)
    vc = v.reshape(B, H, N, RET_CHUNK, Dv).astype(jnp.float32)
    pos = jnp.arange(RET_CHUNK, dtype=jnp.float32)
    rel = pos[:, None] - pos[None, :]
    mask = (rel > 0) if strict else (rel >= 0)
    lg = log_gamma[:, None, None]
    decay = jnp.where(mask, jnp.exp(lg * jnp.where(mask, rel, 0.0)), 0.0)
    scores = jnp.einsum('bhnqd,bhnkd->bhnqk', qc, kc) * decay[None, :, None]
    y = jnp.einsum('bhnqk,bhnkv->bhnqv', scores, vc)
    zeta = jnp.exp(log_gamma[:, None] * (RET_CHUNK - 1 - pos)[None, :])
    u = jnp.einsum('bhnkd,bhnkv->bhndv', kc * zeta[None, :, None, :, None], vc)
    chunk_decay = jnp.exp(log_gamma * RET_CHUNK)[None, :, None, None]

    def step(state, u_i):
        return state * chunk_decay + u_i, state

    _, s_prev = lax.scan(step, jnp.zeros((B, H, Dk, Dv), jnp.float32), jnp.moveaxis(u, 2, 0))
    s_prev = jnp.moveaxis(s_prev, 0, 2)
    xi = jnp.exp(log_gamma[:, None] * (pos + 1.0)[None, :])
    y = y + jnp.einsum('bhnqd,bhndv->bhnqv', qc, s_prev) * xi[None, :, None, :, None]
    return y.reshape(B, H, S, Dv)


def expert_choice_ffn(h, w_router, w_gate, w_up, w_down):
    B, S, D = h.shape
    cap = CAPACITY_FACTOR * S // N_EXPERTS
    logits = jnp.einsum('bsd,de->bse', h, w_router).astype(jnp.float32)
    aff = jax.nn.softmax(logits, axis=-1)
    g, idx = lax.top_k(jnp.swapaxes(aff, 1, 2), cap)
    xin = jax.vmap(lambda hb, ib: hb[ib])(h, idx)
    a = jnp.einsum('becd,edf->becf', xin, w_gate)
    b = jnp.einsum('becd,edf->becf', xin, w_up)
    y = jnp.einsum('becf,efd->becd', jax.nn.silu(a) * b, w_down)
    y = y * g[..., None].astype(y.dtype)
    return jax.vmap(lambda yb, ib: jnp.zeros((S, D), yb.dtype).at[ib.reshape(-1)].add(yb.reshape(-1, D)))(y, idx)


def setup_inputs(seed: int = 0) -> dict:
    key = jax.random.key(seed)
    ks = jax.random.split(key, 16)
    f32 = jnp.float32
    D = D_MODEL
    x = jax.random.normal(ks[0], (BATCH, SEQ, D), f32)
    w_in = jax.random.normal(ks[1], (DEPTH, D, N_IN), f32) * D ** -0.5
    w_attn_out = jax.random.normal(ks[2], (DEPTH, ATTN_WIDTH, D), f32) * ATTN_WIDTH ** -0.5
    w_ret_out = jax.random.normal(ks[3], (DEPTH, RET_WIDTH, D), f32) * RET_WIDTH ** -0.5
    w_out = jax.random.normal(ks[4], (DEPTH, D, D), f32) * D ** -0.5
    base = jnp.log(2.0 ** (5.0 + jnp.arange(RET_HEADS, dtype=f32)) - 1.0)
    ret_decay_logit = base[None, None, :] + 0.1 * jax.random.normal(ks[5], (DEPTH, 2, RET_HEADS), f32)
    norm_mix = 1.0 + 0.05 * jax.random.normal(ks[6], (DEPTH, D), f32)
    norm_ffn = 1.0 + 0.05 * jax.random.normal(ks[7], (DEPTH, D), f32)
    w_router = jax.random.normal(ks[8], (DEPTH, D, N_EXPERTS), f32) * D ** -0.5
    w_gate = jax.random.normal(ks[9], (DEPTH, N_EXPERTS, D, EXPERT_FF), f32) * D ** -0.5
    w_up = jax.random.normal(ks[10], (DEPTH, N_EXPERTS, D, EXPERT_FF), f32) * D ** -0.5
    w_down = jax.random.normal(ks[11], (DEPTH, N_EXPERTS, EXPERT_FF, D), f32) * EXPERT_FF ** -0.5
    norm_final = 1.0 + 0.05 * jax.random.normal(ks[12], (D,), f32)
    return {"x": x, "w_in": w_in, "w_attn_out": w_attn_out, "w_ret_out": w_ret_out,
            "w_out": w_out, "ret_decay_logit": ret_decay_logit, "norm_mix": norm_mix,
            "norm_ffn": norm_ffn, "w_router": w_router, "w_gate": w_gate, "w_up": w_up,
            "w_down": w_down, "norm_final": norm_final}


def reference(x, w_in, w_attn_out, w_ret_out, w_out, ret_decay_logit, norm_mix, norm_ffn,
              w_router, w_gate, w_up, w_down, norm_final):
    B, S, D = x.shape
    slopes = alibi_slopes(ATTN_HEADS)
    for layer in range(DEPTH):
        h = rms_norm(x, norm_mix[layer])
        proj = jnp.einsum('bsd,dn->bsn', h, w_in[layer])
        a_qkv, r_q, r_k, r_v, r_g, gates = jnp.split(proj, SPLITS, axis=-1)

        a_qkv = a_qkv.reshape(B, S, N_ATTN_GROUPS, 3, ATTN_HEADS, ATTN_HEAD_DIM)
        outs, lses = [], []
        for gi, (window, dilation) in enumerate(ATTN_PATTERNS):
            half = window // (2 * dilation)
            o, lse = dilated_band_attention(a_qkv[:, :, gi, 0], a_qkv[:, :, gi, 1], a_qkv[:, :, gi, 2],
                                            dilation, half, slopes)
            outs.append(o)
            lses.append(lse)
        alpha = jax.nn.softmax(jnp.stack(lses, axis=0), axis=0)
        attn = sum(alpha[gi][..., None] * outs[gi] for gi in range(N_ATTN_GROUPS))
        attn = attn.reshape(B, S, ATTN_WIDTH).astype(x.dtype)

        def heads(t):
            return t.reshape(B, S, RET_HEADS, RET_HEAD_DIM).transpose(0, 2, 1, 3)
        q, k, v = heads(r_q), heads(r_k), heads(r_v)
        log_gamma = jax.nn.log_sigmoid(ret_decay_logit[layer].astype(jnp.float32))
        fwd = retention_chunkwise(q, k, v, log_gamma[0], strict=False)
        bwd = jnp.flip(retention_chunkwise(jnp.flip(q, 2), jnp.flip(k, 2), jnp.flip(v, 2),
                                           log_gamma[1], strict=True), 2)
        r = fwd + bwd
        r = r * lax.rsqrt(jnp.mean(r * r, axis=-1, keepdims=True) + EPS)
        r = r.transpose(0, 2, 1, 3).reshape(B, S, RET_WIDTH)
        ret = (jax.nn.silu(r_g.astype(jnp.float32)) * r).astype(x.dtype)

        g_a, g_r = jnp.split(gates, 2, axis=-1)
        merged = (jax.nn.sigmoid(g_a) * jnp.einsum('bsw,wd->bsd', attn, w_attn_out[layer])
                  + jax.nn.sigmoid(g_r) * jnp.einsum('bsw,wd->bsd', ret, w_ret_out[layer]))
        x = x + jnp.einsum('bsd,de->bse', merged, w_out[layer])

        h2 = rms_norm(x, norm_ffn[layer])
        x = x + expert_choice_ffn(h2, w_router[layer], w_gate[layer], w_up[layer], w_down[layer])
    return rms_norm(x, norm_final)
```

```python
import contextlib
import numpy as np
import concourse.bass as bass
import concourse.mybir as mybir
from concourse.bass_utils import run_bass_kernel_spmd

F32 = mybir.dt.float32
BF16 = mybir.dt.bfloat16
AF = mybir.ActivationFunctionType
ALU = mybir.AluOpType

T = 2048
D = 1024
NT = 16
DEPTH = 2
N_IN = 10752
EPS = 1e-6
DILS = (1, 4, 16)
ENGS = ("pe", "act", "dve", "pool", "sp")


class Buf:
    __slots__ = ("name", "lw", "rd", "dsem", "dcnt", "excl")

    def __init__(self, name, excl=False):
        self.name = name
        self.excl = excl
        self.lw = None
        self.rd = {}
        self.dsem = None
        self.dcnt = 0


class _Rec:
    def __init__(self):
        self.calls = []

    def __getattr__(self, name):
        def m(*a, **k):
            self.calls.append((name, a, k))
            return len(self.calls) - 1
        return m


class Prog:
    def __init__(self, nc):
        self.nc = nc
        self.q = {e: [] for e in ENGS}
        self.seq = {e: 0 for e in ENGS}
        self.waited = {e: {} for e in ENGS}
        self.dcount = {}
        self.disabled = False
        self.free_dsems = {}

    def _need(self, eng, waits, key, val):
        if key == ("e", "pe") and eng == "pe":
            return
        if val <= self.waited[eng].get(key, 0):
            return
        if waits.get(key, 0) < val:
            waits[key] = val

    def _deps(self, eng, r, w):
        waits = {}
        for b in r:
            if b.lw is not None:
                self._need(eng, waits, *b.lw)
            if b.excl:
                for k, v in b.rd.items():
                    if k != ("e", eng):
                        self._need(eng, waits, k, v)
        for b in w:
            if b.lw is not None:
                self._need(eng, waits, *b.lw)
            for k, v in b.rd.items():
                self._need(eng, waits, k, v)
        for k, v in waits.items():
            self.waited[eng][k] = v
        return waits

    def op(self, eng, fn, r=(), w=()):
        if self.disabled:
            return 0
        waits = self._deps(eng, r, w)
        self.seq[eng] += 1
        n = self.seq[eng]
        rec = _Rec()
        fn(rec)
        self.q[eng].append((waits, rec.calls, ("e", eng)))
        key = ("e", eng)
        for b in r:
            if b.rd.get(key, 0) < n:
                b.rd[key] = n
        for b in w:
            b.lw = (key, n)
            b.rd = {}
        return n

    def _dsem_for(self, b, eng):
        if b.dsem is None:
            fl = self.free_dsems.setdefault(eng, [])
            if fl:
                b.dsem = fl.pop()
            else:
                b.dsem = ("d", len(self.dcount))
                self.dcount[b.dsem] = 0
            b.dcnt = eng
        assert b.dcnt == eng, (b.name, b.dcnt, eng)
        return b.dsem

    def dma(self, eng, fn, r=(), w=(), nd=1):
        if self.disabled:
            return
        bufs = list(w) if w else list(r)
        assert len(bufs) == 1
        b = bufs[0]
        waits = self._deps(eng, r, w)
        key = self._dsem_for(b, eng)
        self.dcount[key] += 16 * nd
        val = self.dcount[key]
        rec = _Rec()
        marks = []
        fn(rec, marks.append)
        assert len(marks) == nd
        self.q[eng].append((waits, (rec.calls, marks), key))
        if w:
            b.lw = (key, val)
            b.rd = {}
            for rb in r:
                rb.rd[key] = val
        else:
            b.rd[key] = val

    def release(self, bufs):
        if self.disabled:
            return
        for b in bufs:
            if b.dsem is not None:
                self.free_dsems.setdefault(b.dcnt, []).append(b.dsem)
                b.dsem = None

    def barrier(self):
        if self.disabled:
            return
        for e in ENGS:
            waits = {}
            for e2 in ENGS:
                if e2 != e and self.seq[e2] > self.waited[e].get(("e", e2), 0):
                    waits[("e", e2)] = self.seq[e2]
            for k, v in self.dcount.items():
                if v > self.waited[e].get(k, 0):
                    waits[k] = v
            for k, v in waits.items():
                self.waited[e][k] = v
            if waits:
                self.q[e].append((waits, None, None))

    def emit(self):
        nc = self.nc
        names = {"pe": "tensor", "act": "scalar", "dve": "vector", "pool": "gpsimd", "sp": "sync"}
        sems = {}
        with contextlib.ExitStack() as st:
            for e in ENGS:
                sems[("e", e)] = st.enter_context(nc.semaphore("s_" + e))
            for k in self.dcount:
                sems[k] = st.enter_context(nc.semaphore("d_%d" % k[1]))
            block = st.enter_context(nc.Block())
            for e in ENGS:
                self._emit_engine(block, e, names[e], sems)

    def _emit_engine(self, block, e, attr, sems):
        q = self.q[e]

        def body(eng):
            for waits, fn, inc in q:
                for k, v in waits.items():
                    eng.wait_ge(sems[k], v)
                if fn is None:
                    continue
                if inc[0] == "e":
                    ins = None
                    for name, a, k in fn:
                        ins = getattr(eng, name)(*a, **k)
                    ins.then_inc(sems[inc], 1)
                else:
                    s = sems[inc]
                    calls, marks = fn
                    for i, (name, a, k) in enumerate(calls):
                        ins = getattr(eng, name)(*a, **k)
                        if i in marks:
                            ins.then_inc(s, 16)

        getattr(block, attr)(body)


class Arena:
    def __init__(self, nc, st, nbytes):
        self.t16 = st.enter_context(nc.sbuf_tensor("arena", [128, nbytes // 2], BF16))
        self.t32 = self.t16.bitcast(F32)
        self.nbytes = nbytes
        self.top = 0
        self.peak = 0

    def alloc(self, nfree, dt):
        esz = 2 if dt == BF16 else 4
        off = (self.top + 63) // 64 * 64
        sz = nfree * esz
        assert off + sz <= self.nbytes, ("arena overflow", off, sz, self.nbytes)
        self.top = off + sz
        self.peak = max(self.peak, self.top)
        if dt == BF16:
            return self.t16[:, off // 2: off // 2 + nfree]
        return self.t32[:, off // 4: off // 4 + nfree]

    def mark(self):
        return self.top

    def reset(self, m):
        self.top = m


def _host_consts():
    c = {}
    c["ident"] = np.eye(128, dtype=np.float32)
    i = np.arange(128)
    c["ustrict"] = (i[:, None] < i[None, :]).astype(np.float32)
    c["ones"] = np.ones((128, 128), np.float32)
    rel = (i[None, :] - i[:, None]).astype(np.float32)
    c["relm"] = rel
    c["ger"] = (rel >= 0).astype(np.float32)
    c["iota256"] = np.tile(np.arange(256, dtype=np.float32)[None, :], (128, 1))
    c["delta"] = np.tile((128.0 * np.arange(16, dtype=np.float32))[None, :], (128, 1))
    tok = np.zeros((128, 16, 2), np.float32)
    tok[:, :, 0] = np.arange(128, dtype=np.float32)[:, None]
    tok[:, :, 1] = np.arange(16, dtype=np.float32)[None, :]
    c["tokid"] = tok.reshape(128, 32)
    slopes = 2.0 ** (-(np.arange(8, dtype=np.float64) + 1.0))
    am = np.zeros((24, 128, 384), np.float32)
    kl = np.arange(128)[:, None]
    ql = np.arange(128)[None, :]
    for g, dil in enumerate(DILS):
        for h in range(8):
            for s in range(3):
                r = (s - 1) * 128 + kl - ql
                v = np.where(np.abs(r) <= 64, np.exp(-slopes[h] * dil * np.abs(r)), 0.0)
                am[g * 8 + h, :, s * 128:(s + 1) * 128] = v
    c["amask"] = am
    return c


class _Stop(Exception):
    pass


def build_program(depth=DEPTH, dbg=(), stop_after=None):
    nc = bass.Bass("TRN2", target_bir_lowering=False)
    din = {}

    def inp(name, shape):
        din[name] = nc.dram_tensor(name, shape, F32, kind="ExternalInput").ap()

    inp("x", [T, D])
    inp("w_in", [DEPTH, D, N_IN])
    inp("w_attn_out", [DEPTH, 512, D])
    inp("w_ret_out", [DEPTH, D, D])
    inp("w_out", [DEPTH, D, D])
    inp("rdl", [1, DEPTH * 8])
    inp("norm_mix", [DEPTH, D])
    inp("norm_ffn", [DEPTH, D])
    inp("norm_final", [1, D])
    inp("w_router", [DEPTH, D, 16])
    inp("w_gate", [DEPTH, 16, D, 2048])
    inp("w_up", [DEPTH, 16, D, 2048])
    inp("w_down", [DEPTH, 16, 2048, D])
    for k, v in _host_consts().items():
        inp("c_" + k, list(v.shape))
    out_d = nc.dram_tensor("out", [T, D], F32, kind="ExternalOutput").ap()
    h2_scr = nc.dram_tensor("h2_scr", [T, D], BF16, kind="Internal").ap()
    moe_acc = nc.dram_tensor("moe_acc", [T, D], F32, kind="Internal").ap()
    dbg_d = {}
    for name, shape in dbg:
        dbg_d[name] = nc.dram_tensor(name, list(shape), F32, kind="ExternalOutput").ap()

    P = Prog(nc)
    st = contextlib.ExitStack()
    with st:
        def sb(name, shape, dt):
            return st.enter_context(nc.sbuf_tensor(name, shape, dt))

        x_sb = sb("x_sb", [128, NT, D], F32)
        xb = [Buf("x%d" % t) for t in range(NT)]
        ident = sb("ident", [128, 128], BF16)
        identf = sb("identf", [128, 128], F32)
        ustrict = sb("ustrict", [128, 128], BF16)
        ones = sb("ones", [128, 128], BF16)
        relm = sb("relm", [128, 128], F32)
        ger = sb("ger", [128, 128], F32)
        iota256 = sb("iota256", [128, 256], F32)
        delta = sb("delta", [128, 16], F32)
        epst = sb("epst", [128, 1], F32)
        tokid = sb("tokid", [128, 32], BF16)
        rdl_sb = sb("rdl_sb", [128, DEPTH * 8], F32)
        small = sb("small", [128, 64], F32)
        cb = Buf("consts")
        smallb = Buf("small")
        arena = Arena(nc, st, 140 * 1024)
        pbank = [st.enter_context(nc.psum_tensor("pb%d" % i, [128, 512], F32)) for i in range(8)]
        pbank16 = [t.bitcast(BF16) for t in pbank]
        PB = [Buf("pb%d" % i, excl=True) for i in range(8)]

        def ld(eng, dst, src, buf):
            P.dma(eng, lambda e, inc: inc(e.dma_start(out=dst, in_=src)), w=[buf])

        def ld_multi(eng, pairs, buf):
            def f(e, inc):
                for dst, src in pairs:
                    inc(e.dma_start(out=dst, in_=src))
            P.dma(eng, f, w=[buf], nd=len(pairs))

        for name, dst in (("ident", ident), ("ustrict", ustrict), ("ones", ones), ("tokid", tokid)):
            ld("pool", dst[:], din["c_" + name], Buf("c_" + name))
        for name, dst in (("ident", identf), ("relm", relm), ("ger", ger), ("iota256", iota256), ("delta", delta)):
            ld("sp", dst[:], din["c_" + name], Buf("cf_" + name))
        rdl_ap = din["rdl"]
        ld("sp", rdl_sb[:], bass.AP(rdl_ap.tensor, rdl_ap.offset, [[0, 128], [1, DEPTH * 8]]), Buf("rdl"))
        P.op("dve", lambda e: e.memset(epst[:], EPS), w=[cb])
        for t in range(NT):
            ld("sp", x_sb[:, t, :], din["x"][t * 128:(t + 1) * 128, :], xb[t])
        P.barrier()

        def dump(name, ap_fn, bufs):
            if name not in dbg_d:
                return
            def f(e, inc):
                src = ap_fn()
                dst = dbg_d[name]
                n = src.shape[1]
                if n > 2048:
                    src = src.rearrange("p (a b) -> p a b", b=2048)
                    dst = dst.rearrange("p (a b) -> p a b", b=2048)
                inc(e.dma_start(out=dst, in_=src))
            P.dma("pool", f, r=[Buf("dbg_" + name)])
            P.barrier()

        def norm_phase(gain_row_ap, hT, hTb, h_tm=None, h_tmb=None, final_out=None):
            m = arena.mark()
            gain_b = arena.alloc(D, F32)
            junk = arena.alloc(D, BF16)
            hb = [arena.alloc(D, BF16) for _ in range(2)] if h_tm is None and final_out is None else None
            ho = [arena.alloc(D, F32) for _ in range(2)] if final_out is not None else None
            gb = Buf("gain")
            junkb = Buf("junk")
            hbb = [Buf("hb0"), Buf("hb1")]
            ld("sp", gain_b, bass.AP(gain_row_ap.tensor, gain_row_ap.offset, [[0, 128], [1, D]]), gb)
            ss = small[:, 0:16]
            sq = small[:, 16:32]
            rstd = small[:, 32:48]
            stb = [Buf("nst%d" % t) for t in range(NT)]
            P.op("dve", lambda e: e.memset(small[:, 0:48], 0.0), w=stb)
            for t in range(NT):
                def f_sq(e, t=t):
                    return e.activation(out=junk, in_=x_sb[:, t, :], func=AF.Square, accum_out=ss[:, t:t + 1])
                P.op("act", f_sq, r=[xb[t]], w=[junkb, stb[t]])

                def f_sqrt(e, t=t):
                    return e.activation(out=sq[:, t:t + 1], in_=ss[:, t:t + 1], func=AF.Sqrt, scale=1.0 / D, bias=epst[:, 0:1])
                P.op("act", f_sqrt, r=[cb], w=[stb[t]])
                P.op("dve", lambda e, t=t: e.reciprocal(out=rstd[:, t:t + 1], in_=sq[:, t:t + 1]), w=[stb[t]])
                if final_out is not None:
                    dst = ho[t % 2]
                    dstb = hbb[t % 2]
                elif h_tm is not None:
                    dst = h_tm[:, t, :]
                    dstb = h_tmb[t]
                else:
                    dst = hb[t % 2]
                    dstb = hbb[t % 2]

                def f_h(e, t=t, dst=dst):
                    return e.scalar_tensor_tensor(out=dst, in0=x_sb[:, t, :], scalar=rstd[:, t:t + 1], in1=gain_b,
                                                  op0=ALU.mult, op1=ALU.mult)
                P.op("dve", f_h, r=[xb[t], stb[t], gb], w=[dstb])
                if h_tm is not None:
                    P.dma("sp", lambda e, inc, t=t, dst=dst: inc(e.dma_start(out=h2_scr[t * 128:(t + 1) * 128, :], in_=dst)), r=[dstb])
                if final_out is not None:
                    def f_st(e, inc, t=t, dst=dst):
                        inc(e.dma_start(out=final_out[t * 128:(t + 1) * 128, :], in_=dst))
                    P.dma("sp", f_st, r=[dstb])
                    continue
                pbi = t % 2

                def f_tr(e, t=t, dst=dst, pbi=pbi):
                    ins = None
                    for c in range(8):
                        ins = e.transpose(pbank16[pbi][:, c * 128:(c + 1) * 128], dst[:, c * 128:(c + 1) * 128], ident[:])
                    return ins
                P.op("pe", f_tr, r=[dstb], w=[PB[pbi]])

                if t % 2 == 0:
                    def f_ev(e, t=t, pbi=pbi):
                        return e.activation(out=hT[:, :, t * 128:(t + 1) * 128],
                                            in_=pbank16[pbi][:, 0:1024].rearrange("p (c f) -> p c f", c=8), func=AF.Copy)
                    P.op("act", f_ev, r=[PB[pbi]], w=[hTb[t // 4]])
                else:
                    def f_ev(e, t=t, pbi=pbi):
                        return e.tensor_copy(out=hT[:, :, t * 128:(t + 1) * 128],
                                             in_=pbank16[pbi][:, 0:1024].rearrange("p (c f) -> p c f", c=8))
                    P.op("dve", f_ev, r=[PB[pbi]], w=[hTb[t // 4]])
            P.barrier()
            arena.reset(m)

        def proj_group(pbi, lhs_fn, rhs_fn, nk, out_ap, rbufs):
            def f(e):
                ins = None
                for c in range(nk):
                    ins = e.matmul(out_ap, lhsT=lhs_fn(c), rhs=rhs_fn(c), start=(c == 0), stop=(c == nk - 1))
                return ins
            P.op("pe", f, r=rbufs, w=[PB[pbi]])

        evac_flip = [0]

        def evac(out_ap, in_ap, rbufs, wbufs, eng=None):
            if eng is None:
                eng = "act" if evac_flip[0] % 2 == 0 else "dve"
                evac_flip[0] += 1
            if eng == "act":
                P.op("act", lambda e: e.activation(out=out_ap, in_=in_ap, func=AF.Copy), r=rbufs, w=wbufs)
            else:
                P.op("dve", lambda e: e.tensor_copy(out=out_ap, in_=in_ap), r=rbufs, w=wbufs)

        def phase_end(name):
            if stop_after == name:
                P.barrier()
                P.disabled = True

        for layer in range(depth):
            w_in = din["w_in"][layer]
            mix_mark = arena.mark()
            hT2d = arena.alloc(8 * T, BF16)
            hT = hT2d.rearrange("p (c t) -> p c t", c=8)
            hTb = [Buf("hT%d" % i) for i in range(4)]
            attnT2d = arena.alloc(4 * T, BF16)
            attnT = attnT2d.rearrange("p (c t) -> p c t", c=4)
            attnTb = Buf("attnT")

            norm_phase(din["norm_mix"][layer:layer + 1, :], hT, hTb)
            phase_end("n1")
            P.barrier()
            if layer == 0:
                dump("d_hT", lambda: hT2d, [hTb[3]])

            m_att = arena.mark()
            masks2d = arena.alloc(24 * 384, BF16)
            masks = masks2d.rearrange("p (g f) -> p g f", g=24)
            maskb = Buf("masks")
            ld("pool", masks, din["c_amask"].rearrange("g p f -> p g f"), maskb)
            wqkv = [arena.alloc(8 * 3 * 128, BF16).rearrange("p (c w f) -> p c w f", c=8, w=3) for _ in range(2)]
            wqkvb = [Buf("wqkv0"), Buf("wqkv1")]
            qT2 = [arena.alloc(T, BF16) for _ in range(2)]
            kT2 = [arena.alloc(T, BF16) for _ in range(2)]
            qT2b = [Buf("qT2_0"), Buf("qT2_1")]
            kT2b = [Buf("kT2_0"), Buf("kT2_1")]
            vaug2d = [arena.alloc(16 * 2 * 128, BF16) for _ in range(2)]
            vaug = [v.rearrange("p (k h f) -> p k h f", k=16, h=2) for v in vaug2d]
            vaugb = [Buf("vaug0"), Buf("vaug1")]
            et = [arena.alloc(384, BF16) for _ in range(2)]
            etb = [Buf("et0"), Buf("et1")]
            pt = [arena.alloc(384, BF16) for _ in range(2)]
            ptb = [Buf("pt0"), Buf("pt1")]
            acc = [arena.alloc(T, F32) for _ in range(2)]
            accb = [Buf("acc0"), Buf("acc1")]
            rec = arena.alloc(T, F32)
            recb = Buf("rec")
            for i in range(2):
                P.op("dve", lambda e, i=i: e.memset(vaug2d[i], 1.0), w=[vaugb[i]])
            its = [(hp, g) for hp in range(4) for g in range(3)]

            def gen_proj(it):
                hp, g = its[it]
                dil = DILS[g]
                slot = it % 2
                col0 = g * 1536 + hp * 128
                ld_multi("pool", [(wqkv[slot][:, :, wh, :],
                                   bass.AP(w_in.tensor, w_in.offset + col0 + wh * 512, [[N_IN, 128], [128 * N_IN, 8], [1, 128]]))
                                  for wh in range(3)], wqkvb[slot])
                for which, dstT, dstb in ((0, qT2[slot], qT2b[slot]), (1, kT2[slot], kT2b[slot])):
                    for tg in range(4):
                        pbi = tg % 2
                        proj_group(pbi, lambda c, which=which: wqkv[slot][:, c, which, :],
                                   lambda c, tg=tg: hT[:, c, tg * 512:(tg + 1) * 512], 8, pbank[pbi][:, :],
                                   [wqkvb[slot], hTb[tg]])
                        if dil == 1:
                            o_ap = dstT[:, tg * 512:(tg + 1) * 512]
                            i_ap = pbank[pbi][:, :]
                        else:
                            lw = 512 // dil
                            o_ap = dstT.rearrange("p (r l) -> p r l", r=dil)[:, :, tg * lw:(tg + 1) * lw]
                            i_ap = pbank[pbi][:, :].rearrange("p (l r) -> p r l", r=dil)
                        evac(o_ap, i_ap, [PB[pbi]], [dstb])
                        yield
                for ktg in range(4):
                    def f_v(e, ktg=ktg):
                        ins = None
                        for kk in range(4):
                            kt = ktg * 4 + kk
                            if dil == 1:
                                t0 = 128 * kt
                            elif dil == 4:
                                t0 = 512 * (kt % 4) + kt // 4
                            else:
                                t0 = kt
                            for c in range(8):
                                lhs = bass.AP(hT2d.tensor, hT2d.offset + c * T + t0, [[hT2d.ap[0][0], 128], [dil, 128]])
                                ins = e.matmul(pbank[2][:, kk * 128:(kk + 1) * 128], lhsT=lhs, rhs=wqkv[slot][:, c, 2, :],
                                               start=(c == 0), stop=(c == 7))
                        return ins
                    P.op("pe", f_v, r=[wqkvb[slot]] + hTb, w=[PB[2]])
                    pv = pbank[2][:, :].rearrange("p (k h f) -> p k h f", k=4, h=2)
                    evac(vaug[slot][:, ktg * 4:(ktg + 1) * 4, 0, 0:64], pv[:, :, 0, :], [PB[2]], [vaugb[slot]], eng="act")
                    evac(vaug[slot][:, ktg * 4:(ktg + 1) * 4, 1, 64:128], pv[:, :, 1, :], [PB[2]], [vaugb[slot]], eng="act")
                    yield

            def gen_core(it):
                hp, g = its[it]
                dil = DILS[g]
                slot = it % 2
                tps = 16 // dil
                for hh in range(2):
                    h = hp * 2 + hh
                    ps = slice(hh * 64, hh * 64 + 64)

                    def kts_of(j):
                        return [kt for kt in (j - 1, j, j + 1) if 0 <= kt < NT and kt // tps == j // tps]

                    def issue_S(j, ps=ps, h=h):
                        kts = kts_of(j)
                        s0 = kts[0] - j + 1
                        s1 = kts[-1] - j + 1
                        sb_i = 3 + j % 2

                        def f_s(e):
                            ins = None
                            for kt in kts:
                                s = kt - j + 1
                                ins = e.matmul(pbank[sb_i][:, s * 128:(s + 1) * 128], lhsT=kT2[slot][ps, kt * 128:(kt + 1) * 128],
                                               rhs=qT2[slot][ps, j * 128:(j + 1) * 128], start=True, stop=True)
                            return ins
                        P.op("pe", f_s, r=[qT2b[slot], kT2b[slot]], w=[PB[sb_i]])
                        es = j % 2
                        rng = slice(s0 * 128, (s1 + 1) * 128)
                        P.op("act", lambda e: e.activation(out=et[es][:, rng], in_=pbank[sb_i][:, rng], func=AF.Exp, scale=0.125),
                             r=[PB[sb_i]], w=[etb[es]])
                        P.op("dve", lambda e: e.tensor_tensor(out=pt[es][:, rng], in0=et[es][:, rng], in1=masks[:, g * 8 + h, rng], op=ALU.mult),
                             r=[etb[es], maskb], w=[ptb[es]])

                    def issue_PV(j, hh=hh):
                        kts = kts_of(j)
                        es = j % 2
                        po_i = 5 + (j // 4) % 2

                        def f_pv(e):
                            ins = None
                            jj = j % 4
                            for i, kt in enumerate(kts):
                                s = kt - j + 1
                                ins = e.matmul(pbank[po_i][:, jj * 128:(jj + 1) * 128], lhsT=vaug[slot][:, kt, hh, :],
                                               rhs=pt[es][:, s * 128:(s + 1) * 128], start=(i == 0), stop=(i == len(kts) - 1))
                            return ins
                        P.op("pe", f_pv, r=[vaugb[slot], ptb[es]], w=[PB[po_i]])
                        if j % 4 == 3:
                            jg = j // 4
                            if dil == 1:
                                a_ap = acc[hh][:, jg * 512:(jg + 1) * 512]
                                p_ap = pbank[po_i][:, :]
                            elif dil == 4:
                                a_ap = acc[hh].rearrange("p (l r) -> p r l", r=4)[:, jg, :]
                                p_ap = pbank[po_i][:, :]
                            else:
                                a_ap = acc[hh].rearrange("p (l r) -> p r l", r=16)[:, jg * 4:(jg + 1) * 4, :]
                                p_ap = pbank[po_i][:, :].rearrange("p (r l) -> p r l", r=4)
                            if g == 0:
                                P.op("dve", lambda e: e.tensor_copy(out=a_ap, in_=p_ap), r=[PB[po_i]], w=[accb[hh]])
                            else:
                                P.op("dve", lambda e: e.tensor_tensor(out=a_ap, in0=a_ap, in1=p_ap, op=ALU.add),
                                     r=[PB[po_i], accb[hh]], w=[accb[hh]])

                    issue_S(0)
                    for j in range(NT):
                        if j + 1 < NT:
                            issue_S(j + 1)
                        issue_PV(j)
                        yield
                if g == 2:
                    P.op("act", lambda e: e.activation(out=rec[0:64, :], in_=acc[0][64:128, :], func=AF.Ln), r=[accb[0]], w=[recb])
                    P.op("act", lambda e: e.activation(out=rec[0:64, :], in_=rec[0:64, :], func=AF.Exp, scale=-1.0), r=[recb], w=[recb])
                    P.op("dve", lambda e: e.tensor_tensor(out=attnT[0:64, hp, :], in0=acc[0][0:64, :], in1=rec[0:64, :], op=ALU.mult),
                         r=[accb[0], recb], w=[attnTb])
                    P.op("act", lambda e: e.activation(out=rec[64:128, :], in_=acc[1][0:64, :], func=AF.Ln), r=[accb[1]], w=[recb])
                    P.op("act", lambda e: e.activation(out=rec[64:128, :], in_=rec[64:128, :], func=AF.Exp, scale=-1.0), r=[recb], w=[recb])
                    P.op("dve", lambda e: e.tensor_tensor(out=attnT[64:128, hp, :], in0=acc[1][64:128, :], in1=rec[64:128, :], op=ALU.mult),
                         r=[accb[1], recb], w=[attnTb])

            for _ in gen_proj(0):
                pass
            for it in range(len(its)):
                g_proj = gen_proj(it + 1) if it + 1 < len(its) else iter(())
                n = 0
                for _ in gen_core(it):
                    n += 1
                    if n % 3 != 0:
                        next(g_proj, None)
                for _ in g_proj:
                    pass
            P.barrier()
            P.release([maskb] + wqkvb)
            phase_end("att")
            arena.reset(m_att)
            if layer == 0:
                dump("d_attnT", lambda: attnT2d, [attnTb])

            retT2d = arena.alloc(8 * T, BF16)
            retT = retT2d.rearrange("p (c t) -> p c t", c=8)
            retTb = Buf("retT")
            m_ret = arena.mark()
            wr2 = [arena.alloc(8 * 256, BF16).rearrange("p (c f) -> p c f", c=8) for _ in range(2)]
            wr2b = [Buf("wrA"), Buf("wrB")]
            rq = arena.alloc(2 * T, BF16).rearrange("p (c t) -> p c t", c=2)
            rk = arena.alloc(2 * T, BF16).rearrange("p (c t) -> p c t", c=2)
            rv = arena.alloc(16 * 256, BF16).rearrange("p (k f) -> p k f", k=16)
            rqb, rkb, rvb = Buf("rq"), Buf("rk"), Buf("rv")
            lg = arena.alloc(16, F32)
            sgm = arena.alloc(8, F32)
            lgb = Buf("lg")
            Z = arena.alloc(4096, BF16)
            Zb = Buf("Z")
            ST = [arena.alloc(512, BF16) for _ in range(3)]
            STb = [Buf("ST%d" % i) for i in range(3)]
            sqr = arena.alloc(2 * 512, BF16).rearrange("p (c f) -> p c f", c=2)
            sqrb = Buf("sqr")
            sgl = arena.alloc(2 * 512, F32).rearrange("p (c f) -> p c f", c=2)
            sglb = Buf("sgl")
            lnv = arena.alloc(512, F32)
            rstd_r = arena.alloc(512, F32)
            lnvb, rstdb = Buf("lnv"), Buf("rstd_r")
            tmpy = arena.alloc(512, F32)
            tmpyb = Buf("tmpy")
            lo = layer * 8
            P.op("act", lambda e: e.activation(out=sgm, in_=rdl_sb[:, lo:lo + 8], func=AF.Sigmoid), w=[lgb])
            P.op("act", lambda e: e.activation(out=lg[:, 0:8], in_=sgm, func=AF.Ln), w=[lgb])
            P.op("dve", lambda e: e.tensor_scalar(out=lg[:, 8:16], in0=lg[:, 0:8], scalar1=-1.0, scalar2=None, op0=ALU.mult), r=[lgb], w=[lgb])
            for h in range(4):
                c0 = 4608 + h * 256
                def ld_w(which, c0=c0):
                    src = bass.AP(w_in.tensor, w_in.offset + c0 + which * 1024, [[N_IN, 128], [128 * N_IN, 8], [1, 256]])
                    ld("pool", wr2[which % 2], src, wr2b[which % 2])
                ld_w(0)
                ld_w(1)
                for zc in range(8):
                    P.op("pool", lambda e, zc=zc: e.iota(lnv, pattern=[[1, 512]], base=zc * 512 - 1920, channel_multiplier=-1,
                                                         allow_small_or_imprecise_dtypes=True), w=[lnvb])
                    P.op("dve", lambda e: e.tensor_scalar(out=rstd_r, in0=lnv, scalar1=0.0, scalar2=None, op0=ALU.max), r=[lnvb], w=[rstdb])
                    P.op("dve", lambda e: e.tensor_scalar(out=tmpy, in0=lnv, scalar1=0.0, scalar2=None, op0=ALU.min), r=[lnvb], w=[tmpyb])
                    P.op("act", lambda e, h=h: e.activation(out=rstd_r, in_=rstd_r, func=AF.Exp, scale=lg[:, h:h + 1]), r=[lgb, rstdb], w=[rstdb])
                    P.op("act", lambda e, h=h: e.activation(out=tmpy, in_=tmpy, func=AF.Exp, scale=lg[:, 8 + 4 + h:8 + 4 + h + 1]), r=[lgb, tmpyb], w=[tmpyb])
                    P.op("dve", lambda e, zc=zc: e.scalar_tensor_tensor(out=Z[:, zc * 512:(zc + 1) * 512], in0=rstd_r, scalar=1.0 / 16.0, in1=tmpy,
                                                                        op0=ALU.mult, op1=ALU.mult), r=[rstdb, tmpyb], w=[Zb])
                for which, dstT, dstb in ((0, rq, rqb), (1, rk, rkb)):
                    for cc in range(2):
                        for tg in range(4):
                            pbi = 6 + tg % 2
                            proj_group(pbi, lambda c, which=which, cc=cc: wr2[which][:, c, cc * 128:(cc + 1) * 128],
                                       lambda c, tg=tg: hT[:, c, tg * 512:(tg + 1) * 512], 8, pbank[pbi][:, :], [wr2b[which], hTb[tg]])
                            evac(dstT[:, cc, tg * 512:(tg + 1) * 512], pbank[pbi][:, :], [PB[pbi]], [dstb])
                ld_w(2)
                ld_w(3)
                for kp in range(8):
                    pbi = 6 + kp % 2

                    def f_rv(e, kp=kp, pbi=pbi):
                        ins = None
                        for kk in range(2):
                            kt = kp * 2 + kk
                            for c in range(8):
                                ins = e.matmul(pbank[pbi][:, kk * 256:(kk + 1) * 256], lhsT=hT[:, c, kt * 128:(kt + 1) * 128],
                                               rhs=wr2[0][:, c, :], start=(c == 0), stop=(c == 7))
                        return ins
                    P.op("pe", f_rv, r=[wr2b[0], hTb[kp // 2]], w=[PB[pbi]])
                    evac(rv[:, kp * 2:kp * 2 + 2, :], pbank[pbi][:, :].rearrange("p (k f) -> p k f", k=2), [PB[pbi]], [rvb])
                for qg in range(4):
                    for cc in range(2):
                        pbi = 6 + cc
                        proj_group(pbi, lambda c, cc=cc: wr2[1][:, c, cc * 128:(cc + 1) * 128],
                                   lambda c, qg=qg: hT[:, c, qg * 512:(qg + 1) * 512], 8, pbank[pbi][:, :], [wr2b[1], hTb[qg]])
                        P.op("act", lambda e, cc=cc, pbi=pbi: e.activation(out=sgl[:, cc, :], in_=pbank[pbi][:, :], func=AF.Silu),
                             r=[PB[pbi]], w=[sglb])

                    def issue_S(kt, qg=qg):
                        pbi = kt % 3

                        def f(e, kt=kt, pbi=pbi):
                            ins = None
                            for cc in range(2):
                                ins = e.matmul(pbank[pbi][:, :], lhsT=rk[:, cc, kt * 128:(kt + 1) * 128],
                                               rhs=rq[:, cc, qg * 512:(qg + 1) * 512], start=(cc == 0), stop=(cc == 1))
                            return ins
                        P.op("pe", f, r=[rqb, rkb], w=[PB[pbi]])
                        off = 128 * (4 * qg - kt + 15)
                        P.op("dve", lambda e, pbi=pbi, off=off: e.tensor_tensor(out=ST[pbi], in0=pbank[pbi][:, :], in1=Z[:, off:off + 512], op=ALU.mult),
                             r=[PB[pbi], Zb], w=[STb[pbi]])

                    def issue_Y(kt):
                        pbi = kt % 3

                        def f(e, kt=kt, pbi=pbi):
                            ins = None
                            for c2 in range(2):
                                ins = e.matmul(pbank[4 + c2][:, :], lhsT=rv[:, kt, c2 * 128:(c2 + 1) * 128], rhs=ST[pbi],
                                               start=(kt == 0), stop=(kt == NT - 1))
                            return ins
                        P.op("pe", f, r=[rvb, STb[pbi]], w=[PB[4], PB[5]])
                    issue_S(0)
                    issue_S(1)
                    for kt in range(NT):
                        issue_Y(kt)
                        if kt + 2 < NT:
                            issue_S(kt + 2)
                    for c2 in range(2):
                        P.op("act", lambda e, c2=c2: e.activation(out=sqr[:, c2, :], in_=pbank[4 + c2][:, :], func=AF.Square),
                             r=[PB[4 + c2]], w=[sqrb])
                    proj_group(3, lambda c: ones[:, :], lambda c: sqr[:, c, :], 2, pbank[3][:, :], [sqrb, cb])
                    P.op("act", lambda e: e.activation(out=lnv, in_=pbank[3][:, :], func=AF.Ln, scale=1.0 / 256.0, bias=epst[:, 0:1]),
                         r=[PB[3], cb], w=[lnvb])
                    P.op("act", lambda e: e.activation(out=rstd_r, in_=lnv, func=AF.Exp, scale=-0.5), r=[lnvb], w=[rstdb])
                    for c2 in range(2):
                        P.op("dve", lambda e, c2=c2: e.tensor_tensor(out=tmpy, in0=pbank[4 + c2][:, :], in1=rstd_r, op=ALU.mult),
                             r=[PB[4 + c2], rstdb], w=[tmpyb])
                        P.op("dve", lambda e, c2=c2, h=h, qg=qg: e.tensor_tensor(
                            out=retT[:, h * 2 + c2, qg * 512:(qg + 1) * 512], in0=tmpy, in1=sgl[:, c2, :], op=ALU.mult),
                            r=[tmpyb, sglb], w=[retTb])
            P.barrier()
            P.release(wr2b)
            phase_end("ret")
            arena.reset(m_ret)
            if layer == 0:
                dump("d_retT", lambda: retT2d, [retTb])

            m_mg = arena.mark()
            mergedT2d = arena.alloc(8 * T, BF16)
            mergedT = mergedT2d.rearrange("p (c t) -> p c t", c=8)
            mergedb = Buf("merged")
            wao = [arena.alloc(4 * 128, BF16).rearrange("p (c f) -> p c f", c=4) for _ in range(2)]
            wro = [arena.alloc(8 * 128, BF16).rearrange("p (c f) -> p c f", c=8) for _ in range(2)]
            wgt = [arena.alloc(8 * 2 * 128, BF16).rearrange("p (c w f) -> p c w f", c=8, w=2) for _ in range(2)]
            waob = [Buf("wao0"), Buf("wao1")]
            wrob = [Buf("wro0"), Buf("wro1")]
            wgtb = [Buf("wgt0"), Buf("wgt1")]
            sga = arena.alloc(512, F32)
            sgr = arena.alloc(512, F32)
            t1 = arena.alloc(512, F32)
            t2 = arena.alloc(512, F32)
            sgab, sgrb, t1b, t2b = Buf("sga"), Buf("sgr"), Buf("t1"), Buf("t2")
            wa_l = din["w_attn_out"][layer]
            wr_l = din["w_ret_out"][layer]
            for c in range(8):
                s = c % 2
                ld("pool", wao[s], bass.AP(wa_l.tensor, wa_l.offset + c * 128, [[D, 128], [128 * D, 4], [1, 128]]), waob[s])
                ld("pool", wro[s], bass.AP(wr_l.tensor, wr_l.offset + c * 128, [[D, 128], [128 * D, 8], [1, 128]]), wrob[s])
                ld_multi("pool", [(wgt[s][:, :, wh, :],
                                   bass.AP(w_in.tensor, w_in.offset + 8704 + wh * 1024 + c * 128, [[N_IN, 128], [128 * N_IN, 8], [1, 128]]))
                                  for wh in range(2)], wgtb[s])
                for tg in range(4):
                    b0 = 4 * (tg % 2)
                    tsl = slice(tg * 512, (tg + 1) * 512)
                    proj_group(b0, lambda k, s=s: wao[s][:, k, :], lambda k, tsl=tsl: attnT[:, k, tsl], 4, pbank[b0][:, :], [waob[s], attnTb])
                    proj_group(b0 + 1, lambda k, s=s: wro[s][:, k, :], lambda k, tsl=tsl: retT[:, k, tsl], 8, pbank[b0 + 1][:, :], [wrob[s], retTb])
                    proj_group(b0 + 2, lambda k, s=s: wgt[s][:, k, 0, :], lambda k, tsl=tsl: hT[:, k, tsl], 8, pbank[b0 + 2][:, :], [wgtb[s], hTb[tg]])
                    proj_group(b0 + 3, lambda k, s=s: wgt[s][:, k, 1, :], lambda k, tsl=tsl: hT[:, k, tsl], 8, pbank[b0 + 3][:, :], [wgtb[s], hTb[tg]])
                    P.op("act", lambda e, b0=b0: e.activation(out=sga, in_=pbank[b0 + 2][:, :], func=AF.Sigmoid), r=[PB[b0 + 2]], w=[sgab])
                    P.op("act", lambda e, b0=b0: e.activation(out=sgr, in_=pbank[b0 + 3][:, :], func=AF.Sigmoid), r=[PB[b0 + 3]], w=[sgrb])
                    P.op("dve", lambda e, b0=b0: e.tensor_tensor(out=t1, in0=pbank[b0][:, :], in1=sga, op=ALU.mult), r=[PB[b0], sgab], w=[t1b])
                    P.op("dve", lambda e, b0=b0: e.tensor_tensor(out=t2, in0=pbank[b0 + 1][:, :], in1=sgr, op=ALU.mult), r=[PB[b0 + 1], sgrb], w=[t2b])
                    P.op("dve", lambda e, c=c, tsl=tsl: e.tensor_tensor(out=mergedT[:, c, tsl], in0=t1, in1=t2, op=ALU.add), r=[t1b, t2b], w=[mergedb])
            P.barrier()
            P.release(waob + wrob + wgtb)
            phase_end("mrg")
            if layer == 0:
                dump("d_mergedT", lambda: mergedT2d, [mergedb])

            arena.reset(mix_mark)
            wo = [arena.alloc(8 * 512, BF16).rearrange("p (c f) -> p c f", c=8) for _ in range(2)]
            wob = [Buf("wo0"), Buf("wo1")]
            wo_l = din["w_out"][layer]
            for cg in range(2):
                ld("pool", wo[cg], bass.AP(wo_l.tensor, wo_l.offset + cg * 512, [[D, 128], [128 * D, 8], [1, 512]]), wob[cg])
            n = 0
            for cg in range(2):
                for t in range(NT):
                    pbi = n % 4
                    n += 1
                    proj_group(pbi, lambda k, t=t: mergedT[:, k, t * 128:(t + 1) * 128], lambda k, cg=cg: wo[cg][:, k, :], 8,
                               pbank[pbi][:, :], [mergedb, wob[cg]])
                    P.op("dve", lambda e, t=t, cg=cg, pbi=pbi: e.tensor_tensor(
                        out=x_sb[:, t, cg * 512:(cg + 1) * 512], in0=x_sb[:, t, cg * 512:(cg + 1) * 512], in1=pbank[pbi][:, :], op=ALU.add),
                        r=[PB[pbi], xb[t]], w=[xb[t]])
            P.barrier()
            P.release(wob)
            phase_end("out")
            arena.reset(mix_mark)
            if layer == 0:
                dump("d_x1", lambda: x_sb[:, :, :].rearrange("p t d -> p (t d)"), [xb[15]])

            moeb = Buf("moe_acc")
            ztile = arena.t32[:, (arena.nbytes - 4096) // 4: arena.nbytes // 4]
            ztb = Buf("ztile")
            P.op("dve", lambda e: e.memset(ztile, 0.0), w=[ztb])
            _zs = bass.AP(ztile.tensor, ztile.offset, [[ztile.ap[0][0], 128], [0, NT], [1, D]])
            P.dma("pool", lambda e, inc: inc(e.dma_start(out=moe_acc.rearrange("(t p) d -> p t d", p=128), in_=_zs)), r=[ztb], w=[moeb])
            afftm = arena.alloc(NT * 16, F32).rearrange("p (t e) -> p t e", t=NT)
            afftmb = Buf("afftm")
            mask_tm = arena.alloc(NT * 16, F32).rearrange("p (t e) -> p t e", t=NT)
            gm_tm = arena.alloc(NT * 16, F32).rearrange("p (t e) -> p t e", t=NT)
            pos_tm = arena.alloc(NT * 16, F32).rearrange("p (t e) -> p t e", t=NT)
            mask16 = arena.alloc(NT * 16, BF16).rearrange("p (t e) -> p t e", t=NT)
            masktb, gmtb, postb, m16b = Buf("mask_tm"), Buf("gm_tm"), Buf("pos_tm"), Buf("mask16")
            gm16_2d = arena.alloc(NT * 16 * 4, BF16)
            gm16 = gm16_2d.rearrange("p (t e two) -> p t e two", t=NT, e=16)
            gmlo = arena.alloc(NT * 16, F32).rearrange("p (t e) -> p t e", t=NT)
            gm16b = Buf("gm16")
            ffn_mark = arena.mark()
            h2tm2d = arena.alloc(NT * D, BF16)
            h2tm = h2tm2d.rearrange("p (t d) -> p t d", t=NT)
            h2tmb = [Buf("h2tm%d" % t) for t in range(NT)]
            hT2d = arena.alloc(8 * T, BF16)
            hT = hT2d.rearrange("p (c t) -> p c t", c=8)
            hTb = [Buf("h2T%d" % i) for i in range(4)]
            norm_phase(din["norm_ffn"][layer:layer + 1, :], hT, hTb, h_tm=h2tm, h_tmb=h2tmb)
            wrt = arena.alloc(8 * 16, BF16).rearrange("p (c e) -> p c e", c=8)
            wrtb = Buf("wrt")
            wr_l2 = din["w_router"][layer]
            ld("pool", wrt, wr_l2.rearrange("(c p) e -> p c e", p=128), wrtb)
            rmax = arena.alloc(NT, F32)
            rsum = arena.alloc(NT, F32)
            rmaxb, rsumb = Buf("rmax"), Buf("rsum")
            affT = arena.alloc(T, F32)
            work = arena.alloc(T, F32)
            maskT16 = arena.alloc(T, BF16)
            affTb, workb, maskT16b = Buf("affT"), Buf("work"), Buf("maskT16")
            m8 = arena.alloc(8, F32)
            m8b = Buf("m8")
            P.op("dve", lambda e: e.memset(rsum, 0.0), w=[rsumb])
            for t in range(NT):
                pbi = t % 2
                proj_group(pbi, lambda c, t=t: hT[:, c, t * 128:(t + 1) * 128], lambda c: wrt[:, c, :], 8, pbank[pbi][:, 0:16], [hTb[t // 4], wrtb])
                P.op("dve", lambda e, t=t, pbi=pbi: e.tensor_reduce(out=rmax[:, t:t + 1], in_=pbank[pbi][:, 0:16], axis=mybir.AxisListType.X,
                                                                   op=ALU.max, negate=True), r=[PB[pbi]], w=[rmaxb])
                P.op("act", lambda e, t=t, pbi=pbi: e.activation(out=afftm[:, t, :], in_=pbank[pbi][:, 0:16], func=AF.Exp,
                                                                 bias=rmax[:, t:t + 1], accum_out=rsum[:, t:t + 1]),
                     r=[PB[pbi], rmaxb], w=[afftmb, rsumb])
            P.op("dve", lambda e: e.reciprocal(out=rsum, in_=rsum), r=[rsumb], w=[rsumb])
            for t in range(NT):
                P.op("dve", lambda e, t=t: e.tensor_scalar(out=afftm[:, t, :], in0=afftm[:, t, :], scalar1=rsum[:, t:t + 1], scalar2=None, op0=ALU.mult),
                     r=[afftmb, rsumb], w=[afftmb])
            for tq in range(4):
                def f_tra(e, tq=tq):
                    ins = None
                    for k in range(4):
                        t = tq * 4 + k
                        ins = e.transpose(pbank[2 + tq % 2][0:16, k * 128:(k + 1) * 128], afftm[:, t, :], identf[:, :])
                    return ins
                P.op("pe", f_tra, r=[afftmb], w=[PB[2 + tq % 2]])
                P.op("act", lambda e, tq=tq: e.activation(out=affT[0:16, tq * 512:(tq + 1) * 512], in_=pbank[2 + tq % 2][0:16, :], func=AF.Copy),
                     r=[PB[2 + tq % 2]], w=[affTb])
            P.op("dve", lambda e: e.tensor_copy(out=work[0:16, :], in_=affT[0:16, :]), r=[affTb], w=[workb])
            for it8 in range(32):
                P.op("dve", lambda e: e.max(out=m8[0:16, :], in_=work[0:16, :]), r=[workb], w=[m8b])
                if it8 < 31:
                    P.op("dve", lambda e: e.match_replace(out=work[0:16, :], in_to_replace=m8[0:16, :], in_values=work[0:16, :], imm_value=-1.0),
                         r=[workb, m8b], w=[workb])
            P.op("dve", lambda e: e.tensor_scalar(out=maskT16[0:16, :], in0=affT[0:16, :], scalar1=m8[0:16, 7:8], scalar2=None, op0=ALU.is_ge),
                 r=[affTb, m8b], w=[maskT16b])

            def f_trm(e):
                ins = None
                for t in range(NT):
                    ins = e.transpose(pbank16[4][:, t * 16:(t + 1) * 16], maskT16[0:16, t * 128:(t + 1) * 128], ident[0:16, 0:16])
                return ins
            P.op("pe", f_trm, r=[maskT16b], w=[PB[4]])
            P.op("act", lambda e: e.activation(out=mask_tm[:, :, :], in_=pbank16[4][:, 0:NT * 16].rearrange("p (t e) -> p t e", t=NT), func=AF.Copy),
                 r=[PB[4]], w=[masktb])
            P.op("dve", lambda e: e.tensor_copy(out=mask16[:, :, :], in_=pbank16[4][:, 0:NT * 16].rearrange("p (t e) -> p t e", t=NT)),
                 r=[PB[4]], w=[m16b])
            P.op("dve", lambda e: e.tensor_tensor(out=gm_tm[:, :, :], in0=mask_tm[:, :, :], in1=afftm[:, :, :], op=ALU.mult), r=[masktb, afftmb], w=[gmtb])
            P.op("dve", lambda e: e.tensor_copy(out=gm16[:, :, :, 0], in_=gm_tm[:, :, :]), r=[gmtb], w=[gm16b])
            P.op("dve", lambda e: e.tensor_tensor(out=gmlo[:, :, :], in0=gm_tm[:, :, :], in1=gm16[:, :, :, 0], op=ALU.subtract), r=[gmtb, gm16b], w=[gm16b])
            P.op("dve", lambda e: e.tensor_copy(out=gm16[:, :, :, 1], in_=gmlo[:, :, :]), r=[gm16b], w=[gm16b])
            _tk = tokid[:, :]
            tok_bc = bass.AP(_tk.tensor, _tk.offset, [[_tk.ap[0][0], 128], [2, NT], [0, 16], [1, 2]])
            P.op("dve", lambda e: e.tensor_copy(out=gm16[:, :, :, 2:4], in_=tok_bc), r=[gm16b], w=[gm16b])
            for t in range(NT):
                pbi = t % 2

                def f_pos(e, t=t, pbi=pbi):
                    ins = None
                    for tp in range(t + 1):
                        lhs = ustrict[:, :] if tp == t else ones[:, :]
                        ins = e.matmul(pbank[pbi][:, 0:16], lhsT=lhs, rhs=mask16[:, tp, :], start=(tp == 0), stop=(tp == t))
                    return ins
                P.op("pe", f_pos, r=[m16b], w=[PB[pbi]])
                P.op("act", lambda e, t=t, pbi=pbi: e.activation(out=pos_tm[:, t, :], in_=pbank[pbi][:, 0:16], func=AF.Copy), r=[PB[pbi]], w=[postb])
            P.barrier()
            P.release([wrtb])
            phase_end("rt")
            if layer == 0:
                dump("d_afftm", lambda: afftm.rearrange("p t e -> p (t e)"), [afftmb])
                dump("d_mask", lambda: mask_tm.rearrange("p t e -> p (t e)"), [masktb])
                dump("d_pos", lambda: pos_tm.rearrange("p t e -> p (t e)"), [postb])
            arena.reset(ffn_mark)
            Sel = [arena.alloc(NT * 256, BF16).rearrange("p (t s) -> p t s", t=NT) for _ in range(2)]
            Selb = [Buf("Sel0"), Buf("Sel1")]
            mtile = [arena.alloc(D, F32) for _ in range(2)]
            mtb = [Buf("mt0"), Buf("mt1")]
            xinT = arena.alloc(8 * 256, BF16).rearrange("p (c s) -> p c s", c=8)
            xinTb = Buf("xinT")
            xin = [arena.alloc(2 * D, BF16).rearrange("p (k d) -> p k d", k=2) for _ in range(2)]
            xinb = [Buf("xin0"), Buf("xin1")]
            gsraw = [arena.alloc(8, F32) for _ in range(2)]
            gsrawb = [Buf("gsraw0"), Buf("gsraw1")]
            gs2 = [arena.alloc(2, F32) for _ in range(2)]
            gs2b = [Buf("gs0"), Buf("gs1")]
            idxf = [arena.alloc(2, F32) for _ in range(2)]
            idxi = [arena.alloc(2, F32).bitcast(mybir.dt.int32) for _ in range(2)]
            idxb = [Buf("idx0"), Buf("idx1")]
            actT = arena.alloc(16 * 256, BF16).rearrange("p (f s) -> p f s", f=16)
            actTb = Buf("actT")
            ysb = arena.alloc(2 * D, F32).rearrange("p (k d) -> p k d", k=2)
            ysbb = Buf("ysb")
            sa = [arena.alloc(256, F32) for _ in range(2)]
            sab = [Buf("sa0"), Buf("sa1")]
            NSLOT = 3
            wg = [arena.alloc(8 * 512, BF16).rearrange("p (c f) -> p c f", c=8) for _ in range(NSLOT)]
            wu = [arena.alloc(8 * 512, BF16).rearrange("p (c f) -> p c f", c=8) for _ in range(NSLOT)]
            wd = [arena.alloc(4 * D, BF16).rearrange("p (c d) -> p c d", c=4) for _ in range(NSLOT)]
            wgb = [Buf("wg%d" % i) for i in range(NSLOT)]
            wub = [Buf("wu%d" % i) for i in range(NSLOT)]
            wdb = [Buf("wd%d" % i) for i in range(NSLOT)]
            slab_ctr = [0]

            def gen_sel(ex, sl):
                for t in range(NT):
                    P.op("dve", lambda e, t=t: e.tensor_scalar(out=Sel[sl][:, t, :], in0=iota256[:, :], scalar1=pos_tm[:, t, ex:ex + 1],
                                                              scalar2=mask_tm[:, t, ex:ex + 1], op0=ALU.is_equal, op1=ALU.mult),
                         r=[postb, masktb], w=[Selb[sl]])
                    if t % 2 == 1:
                        yield
                S = Sel[sl]
                Sb = Selb[sl]
                def f_gs(e):
                    ins = None
                    for kk in range(2):
                        for t in range(NT):
                            ins = e.matmul(pbank[7][:, kk * 4:(kk + 1) * 4], lhsT=S[:, t, kk * 128:(kk + 1) * 128], rhs=gm16[:, t, ex, :],
                                           start=(t == 0), stop=(t == NT - 1))
                    return ins
                P.op("pe", f_gs, r=[Sb, gm16b], w=[PB[7]])
                P.op("act", lambda e: e.activation(out=gsraw[sl], in_=pbank[7][:, 0:8], func=AF.Copy), r=[PB[7]], w=[gsrawb[sl]])
                raw = gsraw[sl].rearrange("p (k f) -> p k f", k=2)
                P.op("dve", lambda e: e.tensor_tensor(out=gs2[sl], in0=raw[:, :, 0], in1=raw[:, :, 1], op=ALU.add), r=[gsrawb[sl]], w=[gs2b[sl]])
                P.op("dve", lambda e: e.scalar_tensor_tensor(out=idxf[sl], in0=raw[:, :, 3], scalar=128.0, in1=raw[:, :, 2], op0=ALU.mult, op1=ALU.add),
                     r=[gsrawb[sl]], w=[idxb[sl]])
                P.op("dve", lambda e: e.tensor_copy(out=idxi[sl], in_=idxf[sl]), r=[idxb[sl]], w=[idxb[sl]])
                yield

            def issue_gather(sl):
                def f_gather(e, inc):
                    for kk in range(2):
                        inc(e.indirect_dma_start(out=xin[sl][:, kk, :], out_offset=None, in_=h2_scr,
                                                 in_offset=bass.IndirectOffsetOnAxis(ap=idxi[sl][:, kk:kk + 1], axis=0)))
                P.dma("pool", f_gather, r=[idxb[sl]], w=[xinb[sl]], nd=2)

            LOOKAHEAD = NSLOT - 1

            def issue_load(n):
                if n >= 64:
                    return
                ex_, sl4_ = n // 4, n % 4
                s = n % NSLOT
                f0 = sl4_ * 512
                wg_l = din["w_gate"][layer, ex_]
                wu_l = din["w_up"][layer, ex_]
                wd_l = din["w_down"][layer, ex_]
                ld("pool", wg[s], bass.AP(wg_l.tensor, wg_l.offset + f0, [[2048, 128], [128 * 2048, 8], [1, 512]]), wgb[s])
                ld("pool", wu[s], bass.AP(wu_l.tensor, wu_l.offset + f0, [[2048, 128], [128 * 2048, 8], [1, 512]]), wub[s])
                ld("pool", wd[s], bass.AP(wd_l.tensor, wd_l.offset + f0 * D, [[D, 128], [128 * D, 4], [1, D]]), wdb[s])

            def gen_main(ex, sl):
                for kk in range(2):
                    pbi = 6 + (kk + 1) % 2

                    def f_xt(e, kk=kk, pbi=pbi):
                        ins = None
                        for c in range(8):
                            ins = e.transpose(pbank16[pbi][:, c * 128:(c + 1) * 128], xin[sl][:, kk, c * 128:(c + 1) * 128], ident[:, :])
                        return ins
                    P.op("pe", f_xt, r=[xinb[sl]], w=[PB[pbi]])
                    evac(xinT[:, :, kk * 128:(kk + 1) * 128], pbank16[pbi][:, 0:1024].rearrange("p (c f) -> p c f", c=8), [PB[pbi]], [xinTb])
                yield False
                for sl4 in range(4):
                    n_slab = ex * 4 + sl4
                    s = n_slab % NSLOT
                    issue_load(n_slab + LOOKAHEAD)
                    if sl4 == 3:
                        yield "last_load"
                    for fc in range(4):
                        fch = sl4 * 4 + fc
                        pbi = 4 + fch % 2

                        def f_ab(e, s=s, fc=fc, pbi=pbi):
                            ins = None
                            for c in range(8):
                                ins = e.matmul(pbank[pbi][:, 0:256], lhsT=wg[s][:, c, fc * 128:(fc + 1) * 128], rhs=xinT[:, c, :],
                                               start=(c == 0), stop=(c == 7))
                            for c in range(8):
                                ins = e.matmul(pbank[pbi][:, 256:512], lhsT=wu[s][:, c, fc * 128:(fc + 1) * 128], rhs=xinT[:, c, :],
                                               start=(c == 0), stop=(c == 7))
                            return ins
                        P.op("pe", f_ab, r=[wgb[s], wub[s], xinTb], w=[PB[pbi]])
                        si = fch % 2
                        P.op("act", lambda e, si=si, pbi=pbi: e.activation(out=sa[si], in_=pbank[pbi][:, 0:256], func=AF.Silu), r=[PB[pbi]], w=[sab[si]])
                        P.op("dve", lambda e, si=si, pbi=pbi, fch=fch: e.tensor_tensor(out=actT[:, fch, :], in0=sa[si], in1=pbank[pbi][:, 256:512], op=ALU.mult),
                             r=[sab[si], PB[pbi]], w=[actTb])
                        yield True
                    def f_dn(e, s=s, sl4=sl4):
                        ins = None
                        for k in range(2):
                            for dg in range(2):
                                for fc in range(4):
                                    fch = sl4 * 4 + fc
                                    ins = e.matmul(pbank[k * 2 + dg][:, :], lhsT=actT[:, fch, k * 128:(k + 1) * 128], rhs=wd[s][:, fc, dg * 512:(dg + 1) * 512],
                                                   start=(fch == 0), stop=(fch == 15))
                        return ins
                    P.op("pe", f_dn, r=[actTb, wdb[s]], w=[PB[0], PB[1], PB[2], PB[3]])
                    yield True
                n = 0
                for k in range(2):
                    for dg in range(2):
                        bi = k * 2 + dg
                        if n % 2 == 0:
                            P.op("act", lambda e, k=k, dg=dg, bi=bi: e.activation(out=ysb[:, k, dg * 512:(dg + 1) * 512], in_=pbank[bi][:, :],
                                                                                func=AF.Copy, scale=gs2[sl][:, k:k + 1]), r=[PB[bi], gs2b[sl]], w=[ysbb])
                        else:
                            P.op("dve", lambda e, k=k, dg=dg, bi=bi: e.tensor_scalar(out=ysb[:, k, dg * 512:(dg + 1) * 512], in0=pbank[bi][:, :],
                                                                                   scalar1=gs2[sl][:, k:k + 1], scalar2=None, op0=ALU.mult),
                                 r=[PB[bi], gs2b[sl]], w=[ysbb])
                        n += 1
                def f_scat(e, inc):
                    for kk in range(2):
                        inc(e.indirect_dma_start(out=moe_acc, out_offset=bass.IndirectOffsetOnAxis(ap=idxi[sl][:, kk:kk + 1], axis=0),
                                                 in_=ysb[:, kk, :], in_offset=None, compute_op=ALU.add))
                P.dma("pool", f_scat, r=[ysbb, idxb[sl]], w=[moeb], nd=2)
                yield False

            for n0 in range(LOOKAHEAD):
                issue_load(n0)
            for _ in gen_sel(0, 0):
                pass
            issue_gather(0)
            for ex in range(16):
                g_sel = gen_sel(ex + 1, (ex + 1) % 2) if ex + 1 < 16 else iter(())
                for ok in gen_main(ex, ex % 2):
                    if ok == "last_load":
                        for _ in g_sel:
                            pass
                        if ex + 1 < 16:
                            issue_gather((ex + 1) % 2)
                    elif ok:
                        next(g_sel, None)
            for t in range(NT):
                mt = mtile[t % 2]
                P.dma("sp", lambda e, inc, t=t, mt=mt: inc(e.dma_start(out=mt, in_=moe_acc[t * 128:(t + 1) * 128, :])), r=[moeb], w=[mtb[t % 2]])
                P.op("dve", lambda e, t=t, mt=mt: e.tensor_tensor(out=x_sb[:, t, :], in0=x_sb[:, t, :], in1=mt, op=ALU.add), r=[mtb[t % 2], xb[t]], w=[xb[t]])
            P.barrier()
            P.release(wgb + wub + wdb)
            phase_end("exp")
            arena.reset(mix_mark)
            if layer == 0:
                dump("d_x2", lambda: x_sb[:, :, :].rearrange("p t d -> p (t d)"), [xb[15]])

        norm_phase(din["norm_final"][0:1, :], None, None, final_out=out_d)
        P.barrier()
        P.disabled = False
        P.barrier()
        print("arena peak bytes/partition:", arena.peak, "dma sems:", len(P.dcount),
              "instr:", {e: len(P.q[e]) for e in ENGS})
        P.emit()
    return nc


_CACHE = {}


def _get_program():
    if "nc" not in _CACHE:
        _CACHE["nc"] = build_program()
    return _CACHE["nc"]


def make_in_maps(inputs, nb=8):
    c = _host_consts()
    shared = {
        "w_in": np.ascontiguousarray(inputs["w_in"], dtype=np.float32),
        "w_attn_out": np.ascontiguousarray(inputs["w_attn_out"], dtype=np.float32),
        "w_ret_out": np.ascontiguousarray(inputs["w_ret_out"], dtype=np.float32),
        "w_out": np.ascontiguousarray(inputs["w_out"], dtype=np.float32),
        "rdl": np.ascontiguousarray(inputs["ret_decay_logit"], dtype=np.float32).reshape(1, DEPTH * 8),
        "norm_mix": np.ascontiguousarray(inputs["norm_mix"], dtype=np.float32),
        "norm_ffn": np.ascontiguousarray(inputs["norm_ffn"], dtype=np.float32),
        "norm_final": np.ascontiguousarray(inputs["norm_final"], dtype=np.float32).reshape(1, D),
        "w_router": np.ascontiguousarray(inputs["w_router"], dtype=np.float32),
        "w_gate": np.ascontiguousarray(inputs["w_gate"], dtype=np.float32),
        "w_up": np.ascontiguousarray(inputs["w_up"], dtype=np.float32),
        "w_down": np.ascontiguousarray(inputs["w_down"], dtype=np.float32),
    }
    for k, v in c.items():
        shared["c_" + k] = v
    x = np.asarray(inputs["x"], dtype=np.float32)
    maps = []
    for b in range(nb):
        m = dict(shared)
        m["x"] = np.ascontiguousarray(x[b])
        maps.append(m)
    return maps


def kernel(**inputs):
    nc = _get_program()
    in_maps = make_in_maps(inputs)
    res = run_bass_kernel_spmd(nc, in_maps, core_ids=list(range(8)))
    return np.stack([np.asarray(r["out"], dtype=np.float32) for r in res.results], axis=0)
```

```python
import contextlib
import numpy as np
import concourse.bass as bass
import concourse.mybir as mybir
from concourse.bass_utils import run_bass_kernel_spmd

F32 = mybir.dt.float32
BF16 = mybir.dt.bfloat16
AF = mybir.ActivationFunctionType
ALU = mybir.AluOpType

T = 2048
D = 1024
NT = 16
DEPTH = 2
N_IN = 10752
EPS = 1e-6
DILS = (1, 4, 16)
ENGS = ("pe", "act", "dve", "pool", "sp")


class Buf:
    __slots__ = ("name", "lw", "rd", "dsem", "dcnt", "excl")

    def __init__(self, name, excl=False):
        self.name = name
        self.excl = excl
        self.lw = None
        self.rd = {}
        self.dsem = None
        self.dcnt = 0


class _Rec:
    def __init__(self):
        self.calls = []

    def __getattr__(self, name):
        def m(*a, **k):
            self.calls.append((name, a, k))
            return len(self.calls) - 1
        return m


class Prog:
    def __init__(self, nc):
        self.nc = nc
        self.q = {e: [] for e in ENGS}
        self.seq = {e: 0 for e in ENGS}
        self.waited = {e: {} for e in ENGS}
        self.dcount = {}
        self.disabled = False
        self.free_dsems = {}

    def _need(self, eng, waits, key, val):
        if key == ("e", "pe") and eng == "pe":
            return
        if val <= self.waited[eng].get(key, 0):
            return
        if waits.get(key, 0) < val:
            waits[key] = val

    def _deps(self, eng, r, w):
        waits = {}
        for b in r:
            if b.lw is not None:
                self._need(eng, waits, *b.lw)
            if b.excl:
                for k, v in b.rd.items():
                    if k != ("e", eng):
                        self._need(eng, waits, k, v)
        for b in w:
            if b.lw is not None:
                self._need(eng, waits, *b.lw)
            for k, v in b.rd.items():
                self._need(eng, waits, k, v)
        for k, v in waits.items():
            self.waited[eng][k] = v
        return waits

    def op(self, eng, fn, r=(), w=()):
        if self.disabled:
            return 0
        waits = self._deps(eng, r, w)
        self.seq[eng] += 1
        n = self.seq[eng]
        rec = _Rec()
        fn(rec)
        self.q[eng].append((waits, rec.calls, ("e", eng)))
        key = ("e", eng)
        for b in r:
            if b.rd.get(key, 0) < n:
                b.rd[key] = n
        for b in w:
            b.lw = (key, n)
            b.rd = {}
        return n

    def _dsem_for(self, b, eng):
        if b.dsem is None:
            fl = self.free_dsems.setdefault(eng, [])
            if fl:
                b.dsem = fl.pop()
            else:
                b.dsem = ("d", len(self.dcount))
                self.dcount[b.dsem] = 0
            b.dcnt = eng
        assert b.dcnt == eng, (b.name, b.dcnt, eng)
        return b.dsem

    def dma(self, eng, fn, r=(), w=(), nd=1):
        if self.disabled:
            return
        bufs = list(w) if w else list(r)
        assert len(bufs) == 1
        b = bufs[0]
        waits = self._deps(eng, r, w)
        key = self._dsem_for(b, eng)
        self.dcount[key] += 16 * nd
        val = self.dcount[key]
        rec = _Rec()
        marks = []
        fn(rec, marks.append)
        assert len(marks) == nd
        self.q[eng].append((waits, (rec.calls, marks), key))
        if w:
            b.lw = (key, val)
            b.rd = {}
            for rb in r:
                rb.rd[key] = val
        else:
            b.rd[key] = val

    def release(self, bufs):
        if self.disabled:
            return
        for b in bufs:
            if b.dsem is not None:
                self.free_dsems.setdefault(b.dcnt, []).append(b.dsem)
                b.dsem = None

    def barrier(self):
        if self.disabled:
            return
        for e in ENGS:
            waits = {}
            for e2 in ENGS:
                if e2 != e and self.seq[e2] > self.waited[e].get(("e", e2), 0):
                    waits[("e", e2)] = self.seq[e2]
            for k, v in self.dcount.items():
                if v > self.waited[e].get(k, 0):
                    waits[k] = v
            for k, v in waits.items():
                self.waited[e][k] = v
            if waits:
                self.q[e].append((waits, None, None))

    def emit(self):
        nc = self.nc
        names = {"pe": "tensor", "act": "scalar", "dve": "vector", "pool": "gpsimd", "sp": "sync"}
        sems = {}
        with contextlib.ExitStack() as st:
            for e in ENGS:
                sems[("e", e)] = st.enter_context(nc.semaphore("s_" + e))
            for k in self.dcount:
                sems[k] = st.enter_context(nc.semaphore("d_%d" % k[1]))
            block = st.enter_context(nc.Block())
            for e in ENGS:
                self._emit_engine(block, e, names[e], sems)

    def _emit_engine(self, block, e, attr, sems):
        q = self.q[e]

        def body(eng):
            for waits, fn, inc in q:
                for k, v in waits.items():
                    eng.wait_ge(sems[k], v)
                if fn is None:
                    continue
                if inc[0] == "e":
                    ins = None
                    for name, a, k in fn:
                        ins = getattr(eng, name)(*a, **k)
                    ins.then_inc(sems[inc], 1)
                else:
                    s = sems[inc]
                    calls, marks = fn
                    for i, (name, a, k) in enumerate(calls):
                        ins = getattr(eng, name)(*a, **k)
                        if i in marks:
                            ins.then_inc(s, 16)

        getattr(block, attr)(body)


class Arena:
    def __init__(self, nc, st, nbytes):
        self.t16 = st.enter_context(nc.sbuf_tensor("arena", [128, nbytes // 2], BF16))
        self.t32 = self.t16.bitcast(F32)
        self.nbytes = nbytes
        self.top = 0
        self.peak = 0

    def alloc(self, nfree, dt):
        esz = 2 if dt == BF16 else 4
        off = (self.top + 63) // 64 * 64
        sz = nfree * esz
        assert off + sz <= self.nbytes, ("arena overflow", off, sz, self.nbytes)
        self.top = off + sz
        self.peak = max(self.peak, self.top)
        if dt == BF16:
            return self.t16[:, off // 2: off // 2 + nfree]
        return self.t32[:, off // 4: off // 4 + nfree]

    def mark(self):
        return self.top

    def reset(self, m):
        self.top = m


def _host_consts():
    c = {}
    c["ident"] = np.eye(128, dtype=np.float32)
    i = np.arange(128)
    c["ustrict"] = (i[:, None] < i[None, :]).astype(np.float32)
    c["ones"] = np.ones((128, 128), np.float32)
    rel = (i[None, :] - i[:, None]).astype(np.float32)
    c["relm"] = rel
    c["ger"] = (rel >= 0).astype(np.float32)
    c["iota256"] = np.tile(np.arange(256, dtype=np.float32)[None, :], (128, 1))
    c["delta"] = np.tile((128.0 * np.arange(16, dtype=np.float32))[None, :], (128, 1))
    tok = np.zeros((128, 16, 2), np.float32)
    tok[:, :, 0] = np.arange(128, dtype=np.float32)[:, None]
    tok[:, :, 1] = np.arange(16, dtype=np.float32)[None, :]
    c["tokid"] = tok.reshape(128, 32)
    slopes = 2.0 ** (-(np.arange(8, dtype=np.float64) + 1.0))
    am = np.zeros((24, 128, 256), np.float32)
    kl = np.arange(128)[:, None]
    ql = np.arange(128)[None, :]
    for g, dil in enumerate(DILS):
        for h in range(8):
            blk = []
            for s in range(3):
                r = (s - 1) * 128 + kl - ql
                blk.append(np.where(np.abs(r) <= 64, np.exp(-slopes[h] * dil * np.abs(r)), 0.0))
            assert blk[0][:, 64:].max() == 0.0 and blk[2][:, :64].max() == 0.0
            am[g * 8 + h, :, 0:64] = blk[0][:, 0:64]
            am[g * 8 + h, :, 64:192] = blk[1]
            am[g * 8 + h, :, 192:256] = blk[2][:, 64:128]
    c["amask"] = am
    return c


class _Stop(Exception):
    pass


def build_program(depth=DEPTH, dbg=(), stop_after=None):
    nc = bass.Bass("TRN2", target_bir_lowering=False)
    din = {}

    def inp(name, shape):
        din[name] = nc.dram_tensor(name, shape, F32, kind="ExternalInput").ap()

    inp("x", [T, D])
    inp("w_in", [DEPTH, D, N_IN])
    inp("w_attn_out", [DEPTH, 512, D])
    inp("w_ret_out", [DEPTH, D, D])
    inp("w_out", [DEPTH, D, D])
    inp("rdl", [1, DEPTH * 8])
    inp("norm_mix", [DEPTH, D])
    inp("norm_ffn", [DEPTH, D])
    inp("norm_final", [1, D])
    inp("w_router", [DEPTH, D, 16])
    inp("w_gate", [DEPTH, 16, D, 2048])
    inp("w_up", [DEPTH, 16, D, 2048])
    inp("w_down", [DEPTH, 16, 2048, D])
    for k, v in _host_consts().items():
        inp("c_" + k, list(v.shape))
    out_d = nc.dram_tensor("out", [T, D], F32, kind="ExternalOutput").ap()
    h2_scr = nc.dram_tensor("h2_scr", [T, D], BF16, kind="Internal").ap()
    moe_acc = nc.dram_tensor("moe_acc", [T, D], F32, kind="Internal").ap()
    dbg_d = {}
    for name, shape in dbg:
        dbg_d[name] = nc.dram_tensor(name, list(shape), F32, kind="ExternalOutput").ap()

    P = Prog(nc)
    st = contextlib.ExitStack()
    with st:
        def sb(name, shape, dt):
            return st.enter_context(nc.sbuf_tensor(name, shape, dt))

        x_sb = sb("x_sb", [128, NT, D], F32)
        xb = [Buf("x%d" % t) for t in range(NT)]
        ident = sb("ident", [128, 128], BF16)
        identf = sb("identf", [128, 128], F32)
        ustrict = sb("ustrict", [128, 128], BF16)
        ones = sb("ones", [128, 128], BF16)
        relm = sb("relm", [128, 128], F32)
        ger = sb("ger", [128, 128], F32)
        iota256 = sb("iota256", [128, 256], F32)
        delta = sb("delta", [128, 16], F32)
        epst = sb("epst", [128, 1], F32)
        tokid = sb("tokid", [128, 32], BF16)
        rdl_sb = sb("rdl_sb", [128, DEPTH * 8], F32)
        small = sb("small", [128, 64], F32)
        cb = Buf("consts")
        smallb = Buf("small")
        arena = Arena(nc, st, 140 * 1024)
        pbank = [st.enter_context(nc.psum_tensor("pb%d" % i, [128, 512], F32)) for i in range(8)]
        pbank16 = [t.bitcast(BF16) for t in pbank]
        PB = [Buf("pb%d" % i, excl=True) for i in range(8)]

        def ld(eng, dst, src, buf):
            P.dma(eng, lambda e, inc: inc(e.dma_start(out=dst, in_=src)), w=[buf])

        def ld_multi(eng, pairs, buf):
            def f(e, inc):
                for dst, src in pairs:
                    inc(e.dma_start(out=dst, in_=src))
            P.dma(eng, f, w=[buf], nd=len(pairs))

        for name, dst in (("ident", ident), ("ustrict", ustrict), ("ones", ones), ("tokid", tokid)):
            ld("pool", dst[:], din["c_" + name], Buf("c_" + name))
        for name, dst in (("ident", identf), ("relm", relm), ("ger", ger), ("iota256", iota256), ("delta", delta)):
            ld("sp", dst[:], din["c_" + name], Buf("cf_" + name))
        rdl_ap = din["rdl"]
        ld("sp", rdl_sb[:], bass.AP(rdl_ap.tensor, rdl_ap.offset, [[0, 128], [1, DEPTH * 8]]), Buf("rdl"))
        P.op("dve", lambda e: e.memset(epst[:], EPS), w=[cb])
        for t in range(NT):
            ld("sp", x_sb[:, t, :], din["x"][t * 128:(t + 1) * 128, :], xb[t])
        P.barrier()

        def dump(name, ap_fn, bufs):
            if name not in dbg_d:
                return
            def f(e, inc):
                src = ap_fn()
                dst = dbg_d[name]
                n = src.shape[1]
                if n > 2048:
                    src = src.rearrange("p (a b) -> p a b", b=2048)
                    dst = dst.rearrange("p (a b) -> p a b", b=2048)
                inc(e.dma_start(out=dst, in_=src))
            P.dma("pool", f, r=[Buf("dbg_" + name)])
            P.barrier()

        def norm_phase(gain_row_ap, hT, hTb, h_tm=None, h_tmb=None, final_out=None):
            m = arena.mark()
            gain_b = arena.alloc(D, F32)
            junk = arena.alloc(D, BF16)
            hb = [arena.alloc(D, BF16) for _ in range(2)] if h_tm is None and final_out is None else None
            ho = [arena.alloc(D, F32) for _ in range(2)] if final_out is not None else None
            gb = Buf("gain")
            junkb = Buf("junk")
            hbb = [Buf("hb0"), Buf("hb1")]
            ld("sp", gain_b, bass.AP(gain_row_ap.tensor, gain_row_ap.offset, [[0, 128], [1, D]]), gb)
            ss = small[:, 0:16]
            sq = small[:, 16:32]
            rstd = small[:, 32:48]
            stb = [Buf("nst%d" % t) for t in range(NT)]
            P.op("dve", lambda e: e.memset(small[:, 0:48], 0.0), w=stb)
            for t in range(NT):
                def f_sq(e, t=t):
                    return e.activation(out=junk, in_=x_sb[:, t, :], func=AF.Square, accum_out=ss[:, t:t + 1])
                P.op("act", f_sq, r=[xb[t]], w=[junkb, stb[t]])

                def f_sqrt(e, t=t):
                    return e.activation(out=sq[:, t:t + 1], in_=ss[:, t:t + 1], func=AF.Sqrt, scale=1.0 / D, bias=epst[:, 0:1])
                P.op("act", f_sqrt, r=[cb], w=[stb[t]])
                P.op("dve", lambda e, t=t: e.reciprocal(out=rstd[:, t:t + 1], in_=sq[:, t:t + 1]), w=[stb[t]])
                if final_out is not None:
                    dst = ho[t % 2]
                    dstb = hbb[t % 2]
                elif h_tm is not None:
                    dst = h_tm[:, t, :]
                    dstb = h_tmb[t]
                else:
                    dst = hb[t % 2]
                    dstb = hbb[t % 2]

                def f_h(e, t=t, dst=dst):
                    return e.scalar_tensor_tensor(out=dst, in0=x_sb[:, t, :], scalar=rstd[:, t:t + 1], in1=gain_b,
                                                  op0=ALU.mult, op1=ALU.mult)
                P.op("dve", f_h, r=[xb[t], stb[t], gb], w=[dstb])
                if h_tm is not None:
                    P.dma("sp", lambda e, inc, t=t, dst=dst: inc(e.dma_start(out=h2_scr[t * 128:(t + 1) * 128, :], in_=dst)), r=[dstb])
                if final_out is not None:
                    def f_st(e, inc, t=t, dst=dst):
                        inc(e.dma_start(out=final_out[t * 128:(t + 1) * 128, :], in_=dst))
                    P.dma("sp", f_st, r=[dstb])
                    continue
                pbi = t % 2

                def f_tr(e, t=t, dst=dst, pbi=pbi):
                    ins = None
                    for c in range(8):
                        ins = e.transpose(pbank16[pbi][:, c * 128:(c + 1) * 128], dst[:, c * 128:(c + 1) * 128], ident[:])
                    return ins
                P.op("pe", f_tr, r=[dstb], w=[PB[pbi]])

                if t % 2 == 0:
                    def f_ev(e, t=t, pbi=pbi):
                        return e.activation(out=hT[:, :, t * 128:(t + 1) * 128],
                                            in_=pbank16[pbi][:, 0:1024].rearrange("p (c f) -> p c f", c=8), func=AF.Copy)
                    P.op("act", f_ev, r=[PB[pbi]], w=[hTb[t // 4]])
                else:
                    def f_ev(e, t=t, pbi=pbi):
                        return e.tensor_copy(out=hT[:, :, t * 128:(t + 1) * 128],
                                             in_=pbank16[pbi][:, 0:1024].rearrange("p (c f) -> p c f", c=8))
                    P.op("dve", f_ev, r=[PB[pbi]], w=[hTb[t // 4]])
            P.barrier()
            arena.reset(m)

        def proj_group(pbi, lhs_fn, rhs_fn, nk, out_ap, rbufs):
            def f(e):
                ins = None
                for c in range(nk):
                    ins = e.matmul(out_ap, lhsT=lhs_fn(c), rhs=rhs_fn(c), start=(c == 0), stop=(c == nk - 1))
                return ins
            P.op("pe", f, r=rbufs, w=[PB[pbi]])

        evac_flip = [0]

        def evac(out_ap, in_ap, rbufs, wbufs, eng=None):
            if eng is None:
                eng = "act" if evac_flip[0] % 2 == 0 else "dve"
                evac_flip[0] += 1
            if eng == "act":
                P.op("act", lambda e: e.activation(out=out_ap, in_=in_ap, func=AF.Copy), r=rbufs, w=wbufs)
            else:
                P.op("dve", lambda e: e.tensor_copy(out=out_ap, in_=in_ap), r=rbufs, w=wbufs)

        def phase_end(name):
            if stop_after == name:
                P.barrier()
                P.disabled = True

        for layer in range(depth):
            w_in = din["w_in"][layer]
            mix_mark = arena.mark()
            hT2d = arena.alloc(8 * T, BF16)
            hT = hT2d.rearrange("p (c t) -> p c t", c=8)
            hTb = [Buf("hT%d" % i) for i in range(4)]
            attnT2d = arena.alloc(4 * T, BF16)
            attnT = attnT2d.rearrange("p (c t) -> p c t", c=4)
            attnTb = Buf("attnT")

            norm_phase(din["norm_mix"][layer:layer + 1, :], hT, hTb)
            phase_end("n1")
            P.barrier()
            if layer == 0:
                dump("d_hT", lambda: hT2d, [hTb[3]])

            m_att = arena.mark()
            masks2d = arena.alloc(24 * 256, BF16)
            masks = masks2d.rearrange("p (g f) -> p g f", g=24)
            maskb = Buf("masks")
            ld("pool", masks, din["c_amask"].rearrange("g p f -> p g f"), maskb)
            wqkv = [arena.alloc(8 * 3 * 128, BF16).rearrange("p (c w f) -> p c w f", c=8, w=3) for _ in range(2)]
            wqkvb = [Buf("wqkv0"), Buf("wqkv1")]
            qT2 = [arena.alloc(T, BF16) for _ in range(2)]
            kT2 = [arena.alloc(T, BF16) for _ in range(2)]
            qT2b = [Buf("qT2_0"), Buf("qT2_1")]
            kT2b = [Buf("kT2_0"), Buf("kT2_1")]
            vaug2d = [arena.alloc(16 * 2 * 128, BF16) for _ in range(2)]
            vaug = [v.rearrange("p (k h f) -> p k h f", k=16, h=2) for v in vaug2d]
            vaugb = [Buf("vaug0"), Buf("vaug1")]
            et = [arena.alloc(256, BF16) for _ in range(3)]
            etb = [Buf("et0"), Buf("et1"), Buf("et2")]
            pt = [arena.alloc(256, BF16) for _ in range(3)]
            ptb = [Buf("pt0"), Buf("pt1"), Buf("pt2")]
            acc = [arena.alloc(T, F32) for _ in range(2)]
            accb = [Buf("acc0"), Buf("acc1")]
            rec = arena.alloc(T, F32)
            recb = Buf("rec")
            for i in range(2):
                P.op("dve", lambda e, i=i: e.memset(vaug2d[i], 1.0), w=[vaugb[i]])
            its = [(hp, g) for hp in range(4) for g in range(3)]

            def gen_proj(it):
                hp, g = its[it]
                dil = DILS[g]
                slot = it % 2
                col0 = g * 1536 + hp * 128
                ld_multi("pool", [(wqkv[slot][:, :, wh, :],
                                   bass.AP(w_in.tensor, w_in.offset + col0 + wh * 512, [[N_IN, 128], [128 * N_IN, 8], [1, 128]]))
                                  for wh in range(3)], wqkvb[slot])
                for which, dstT, dstb in ((0, qT2[slot], qT2b[slot]), (1, kT2[slot], kT2b[slot])):
                    for tg in range(4):
                        pbi = tg % 2
                        proj_group(pbi, lambda c, which=which: wqkv[slot][:, c, which, :],
                                   lambda c, tg=tg: hT[:, c, tg * 512:(tg + 1) * 512], 8, pbank[pbi][:, :],
                                   [wqkvb[slot], hTb[tg]])
                        if dil == 1:
                            o_ap = dstT[:, tg * 512:(tg + 1) * 512]
                            i_ap = pbank[pbi][:, :]
                        else:
                            lw = 512 // dil
                            o_ap = dstT.rearrange("p (r l) -> p r l", r=dil)[:, :, tg * lw:(tg + 1) * lw]
                            i_ap = pbank[pbi][:, :].rearrange("p (l r) -> p r l", r=dil)
                        evac(o_ap, i_ap, [PB[pbi]], [dstb])
                        yield
                for ktg in range(4):
                    def f_v(e, ktg=ktg):
                        ins = None
                        for kk in range(4):
                            kt = ktg * 4 + kk
                            if dil == 1:
                                t0 = 128 * kt
                            elif dil == 4:
                                t0 = 512 * (kt % 4) + kt // 4
                            else:
                                t0 = kt
                            for c in range(8):
                                lhs = bass.AP(hT2d.tensor, hT2d.offset + c * T + t0, [[hT2d.ap[0][0], 128], [dil, 128]])
                                ins = e.matmul(pbank[2][:, kk * 128:(kk + 1) * 128], lhsT=lhs, rhs=wqkv[slot][:, c, 2, :],
                                               start=(c == 0), stop=(c == 7))
                        return ins
                    P.op("pe", f_v, r=[wqkvb[slot]] + hTb, w=[PB[2]])
                    pv = pbank[2][:, :].rearrange("p (k h f) -> p k h f", k=4, h=2)
                    evac(vaug[slot][:, ktg * 4:(ktg + 1) * 4, 0, 0:64], pv[:, :, 0, :], [PB[2]], [vaugb[slot]], eng="act")
                    evac(vaug[slot][:, ktg * 4:(ktg + 1) * 4, 1, 64:128], pv[:, :, 1, :], [PB[2]], [vaugb[slot]], eng="act")
                    yield

            def gen_core(it):
                hp, g = its[it]
                dil = DILS[g]
                slot = it % 2
                tps = 16 // dil
                for hh in range(2):
                    h = hp * 2 + hh
                    ps = slice(hh * 64, hh * 64 + 64)

                    def kts_of(j):
                        return [kt for kt in (j - 1, j, j + 1) if 0 <= kt < NT and kt // tps == j // tps]

                    SEG = {0: (0, 64, 0, 64), 1: (64, 192, 0, 128), 2: (192, 256, 64, 128)}

                    def issue_S(j, ps=ps, h=h):
                        kts = kts_of(j)
                        sb_i = (3, 4, 7)[j % 3]

                        def f_s(e):
                            ins = None
                            for kt in kts:
                                c0, c1, q0, q1 = SEG[kt - j + 1]
                                ins = e.matmul(pbank[sb_i][:, c0:c1], lhsT=kT2[slot][ps, kt * 128:(kt + 1) * 128],
                                               rhs=qT2[slot][ps, j * 128 + q0:j * 128 + q1], start=True, stop=True)
                            return ins
                        P.op("pe", f_s, r=[qT2b[slot], kT2b[slot]], w=[PB[sb_i]])
                        es = j % 3
                        rng = slice(SEG[kts[0] - j + 1][0], SEG[kts[-1] - j + 1][1])
                        P.op("act", lambda e: e.activation(out=et[es][:, rng], in_=pbank[sb_i][:, rng], func=AF.Exp, scale=0.125),
                             r=[PB[sb_i]], w=[etb[es]])
                        P.op("dve",
                             lambda e: e.tensor_tensor(out=pt[es][:, rng], in0=et[es][:, rng], in1=masks[:, g * 8 + h, rng], op=ALU.mult),
                             r=[etb[es], maskb], w=[ptb[es]])

                    def issue_PV(j, hh=hh):
                        kts = kts_of(j)
                        es = j % 3
                        po_i = 5 + (j // 4) % 2

                        def f_pv(e):
                            ins = None
                            jj = j % 4
                            for half in range(2):
                                parts = []
                                if half == 0 and (j - 1) in kts:
                                    parts.append((j - 1, 0, 64))
                                parts.append((j, 64 + half * 64, 128 + half * 64))
                                if half == 1 and (j + 1) in kts:
                                    parts.append((j + 1, 192, 256))
                                o0 = jj * 128 + half * 64
                                for i, (kt, c0, c1) in enumerate(parts):
                                    ins = e.matmul(pbank[po_i][:, o0:o0 + 64], lhsT=vaug[slot][:, kt, hh, :], rhs=pt[es][:, c0:c1],
                                                   start=(i == 0), stop=(i == len(parts) - 1))
                            return ins
                        P.op("pe", f_pv, r=[vaugb[slot], ptb[es]], w=[PB[po_i]])
                        if j % 4 == 3:
                            jg = j // 4
                            if dil == 1:
                                a_ap = acc[hh][:, jg * 512:(jg + 1) * 512]
                                p_ap = pbank[po_i][:, :]
                            elif dil == 4:
                                a_ap = acc[hh].rearrange("p (l r) -> p r l", r=4)[:, jg, :]
                                p_ap = pbank[po_i][:, :]
                            else:
                                a_ap = acc[hh].rearrange("p (l r) -> p r l", r=16)[:, jg * 4:(jg + 1) * 4, :]
                                p_ap = pbank[po_i][:, :].rearrange("p (r l) -> p r l", r=4)
                            if g == 0:
                                P.op("dve", lambda e: e.tensor_copy(out=a_ap, in_=p_ap), r=[PB[po_i]], w=[accb[hh]])
                            else:
                                P.op("dve", lambda e: e.tensor_tensor(out=a_ap, in0=a_ap, in1=p_ap, op=ALU.add),
                                     r=[PB[po_i], accb[hh]], w=[accb[hh]])

                    issue_S(0)
                    issue_S(1)
                    for j in range(NT):
                        if j + 2 < NT:
                            issue_S(j + 2)
                        issue_PV(j)
                        yield
                if g == 2:
                    P.op("act", lambda e: e.activation(out=rec[0:64, :], in_=acc[0][64:128, :], func=AF.Ln), r=[accb[0]], w=[recb])
                    P.op("act", lambda e: e.activation(out=rec[0:64, :], in_=rec[0:64, :], func=AF.Exp, scale=-1.0), r=[recb], w=[recb])
                    P.op("dve", lambda e: e.tensor_tensor(out=attnT[0:64, hp, :], in0=acc[0][0:64, :], in1=rec[0:64, :], op=ALU.mult),
                         r=[accb[0], recb], w=[attnTb])
                    P.op("act", lambda e: e.activation(out=rec[64:128, :], in_=acc[1][0:64, :], func=AF.Ln), r=[accb[1]], w=[recb])
                    P.op("act", lambda e: e.activation(out=rec[64:128, :], in_=rec[64:128, :], func=AF.Exp, scale=-1.0), r=[recb], w=[recb])
                    P.op("dve", lambda e: e.tensor_tensor(out=attnT[64:128, hp, :], in0=acc[1][64:128, :], in1=rec[64:128, :], op=ALU.mult),
                         r=[accb[1], recb], w=[attnTb])

            for _ in gen_proj(0):
                pass
            for it in range(len(its)):
                g_proj = gen_proj(it + 1) if it + 1 < len(its) else iter(())
                n = 0
                for _ in gen_core(it):
                    n += 1
                    if n % 3 != 0:
                        next(g_proj, None)
                for _ in g_proj:
                    pass
            P.barrier()
            P.release([maskb] + wqkvb)
            phase_end("att")
            arena.reset(m_att)
            if layer == 0:
                dump("d_attnT", lambda: attnT2d, [attnTb])

            retT2d = arena.alloc(8 * T, BF16)
            retT = retT2d.rearrange("p (c t) -> p c t", c=8)
            retTb = Buf("retT")
            m_ret = arena.mark()
            wr2 = [arena.alloc(8 * 256, BF16).rearrange("p (c f) -> p c f", c=8) for _ in range(2)]
            wr2b = [Buf("wrA"), Buf("wrB")]
            rq = arena.alloc(2 * T, BF16).rearrange("p (c t) -> p c t", c=2)
            rk = arena.alloc(2 * T, BF16).rearrange("p (c t) -> p c t", c=2)
            rv = arena.alloc(16 * 256, BF16).rearrange("p (k f) -> p k f", k=16)
            rqb, rkb, rvb = Buf("rq"), Buf("rk"), Buf("rv")
            lg = arena.alloc(16, F32)
            sgm = arena.alloc(8, F32)
            lgb = Buf("lg")
            Z = arena.alloc(4096, BF16)
            Zb = Buf("Z")
            ST = [arena.alloc(512, BF16) for _ in range(3)]
            STb = [Buf("ST%d" % i) for i in range(3)]
            sqr = arena.alloc(2 * 512, BF16).rearrange("p (c f) -> p c f", c=2)
            sqrb = Buf("sqr")
            sgl = arena.alloc(2 * 512, F32).rearrange("p (c f) -> p c f", c=2)
            sglb = Buf("sgl")
            lnv = arena.alloc(512, F32)
            rstd_r = arena.alloc(512, F32)
            lnvb, rstdb = Buf("lnv"), Buf("rstd_r")
            tmpy = arena.alloc(512, F32)
            tmpyb = Buf("tmpy")
            lo = layer * 8
            P.op("act", lambda e: e.activation(out=sgm, in_=rdl_sb[:, lo:lo + 8], func=AF.Sigmoid), w=[lgb])
            P.op("act", lambda e: e.activation(out=lg[:, 0:8], in_=sgm, func=AF.Ln), w=[lgb])
            P.op("dve", lambda e: e.tensor_scalar(out=lg[:, 8:16], in0=lg[:, 0:8], scalar1=-1.0, scalar2=None, op0=ALU.mult), r=[lgb], w=[lgb])
            for h in range(4):
                c0 = 4608 + h * 256
                def ld_w(which, c0=c0):
                    src = bass.AP(w_in.tensor, w_in.offset + c0 + which * 1024, [[N_IN, 128], [128 * N_IN, 8], [1, 256]])
                    ld("pool", wr2[which % 2], src, wr2b[which % 2])
                ld_w(0)
                ld_w(1)
                for zc in range(8):
                    P.op("pool", lambda e, zc=zc: e.iota(lnv, pattern=[[1, 512]], base=zc * 512 - 1920, channel_multiplier=-1,
                                                         allow_small_or_imprecise_dtypes=True), w=[lnvb])
                    P.op("dve", lambda e: e.tensor_scalar(out=rstd_r, in0=lnv, scalar1=0.0, scalar2=None, op0=ALU.max), r=[lnvb], w=[rstdb])
                    P.op("dve", lambda e: e.tensor_scalar(out=tmpy, in0=lnv, scalar1=0.0, scalar2=None, op0=ALU.min), r=[lnvb], w=[tmpyb])
                    P.op("act", lambda e, h=h: e.activation(out=rstd_r, in_=rstd_r, func=AF.Exp, scale=lg[:, h:h + 1]), r=[lgb, rstdb], w=[rstdb])
                    P.op("act", lambda e, h=h: e.activation(out=tmpy, in_=tmpy, func=AF.Exp, scale=lg[:, 8 + 4 + h:8 + 4 + h + 1]), r=[lgb, tmpyb], w=[tmpyb])
                    P.op("dve", lambda e, zc=zc: e.scalar_tensor_tensor(out=Z[:, zc * 512:(zc + 1) * 512], in0=rstd_r, scalar=1.0 / 16.0, in1=tmpy,
                                                                        op0=ALU.mult, op1=ALU.mult), r=[rstdb, tmpyb], w=[Zb])
                for which, dstT, dstb in ((0, rq, rqb), (1, rk, rkb)):
                    for cc in range(2):
                        for tg in range(4):
                            pbi = 6 + tg % 2
                            proj_group(pbi, lambda c, which=which, cc=cc: wr2[which][:, c, cc * 128:(cc + 1) * 128],
                                       lambda c, tg=tg: hT[:, c, tg * 512:(tg + 1) * 512], 8, pbank[pbi][:, :], [wr2b[which], hTb[tg]])
                            evac(dstT[:, cc, tg * 512:(tg + 1) * 512], pbank[pbi][:, :], [PB[pbi]], [dstb])
                ld_w(2)
                ld_w(3)
                for kp in range(8):
                    pbi = 6 + kp % 2

                    def f_rv(e, kp=kp, pbi=pbi):
                        ins = None
                        for kk in range(2):
                            kt = kp * 2 + kk
                            for c in range(8):
                                ins = e.matmul(pbank[pbi][:, kk * 256:(kk + 1) * 256], lhsT=hT[:, c, kt * 128:(kt + 1) * 128],
                                               rhs=wr2[0][:, c, :], start=(c == 0), stop=(c == 7))
                        return ins
                    P.op("pe", f_rv, r=[wr2b[0], hTb[kp // 2]], w=[PB[pbi]])
                    evac(rv[:, kp * 2:kp * 2 + 2, :], pbank[pbi][:, :].rearrange("p (k f) -> p k f", k=2), [PB[pbi]], [rvb])
                for qg in range(4):
                    for cc in range(2):
                        pbi = 6 + cc
                        proj_group(pbi, lambda c, cc=cc: wr2[1][:, c, cc * 128:(cc + 1) * 128],
                                   lambda c, qg=qg: hT[:, c, qg * 512:(qg + 1) * 512], 8, pbank[pbi][:, :], [wr2b[1], hTb[qg]])
                        P.op("act", lambda e, cc=cc, pbi=pbi: e.activation(out=sgl[:, cc, :], in_=pbank[pbi][:, :], func=AF.Silu),
                             r=[PB[pbi]], w=[sglb])

                    def issue_S(kt, qg=qg):
                        pbi = kt % 3

                        def f(e, kt=kt, pbi=pbi):
                            ins = None
                            for cc in range(2):
                                ins = e.matmul(pbank[pbi][:, :], lhsT=rk[:, cc, kt * 128:(kt + 1) * 128],
                                               rhs=rq[:, cc, qg * 512:(qg + 1) * 512], start=(cc == 0), stop=(cc == 1))
                            return ins
                        P.op("pe", f, r=[rqb, rkb], w=[PB[pbi]])
                        off = 128 * (4 * qg - kt + 15)
                        P.op("dve", lambda e, pbi=pbi, off=off: e.tensor_tensor(out=ST[pbi], in0=pbank[pbi][:, :], in1=Z[:, off:off + 512], op=ALU.mult),
                             r=[PB[pbi], Zb], w=[STb[pbi]])

                    def issue_Y(kt):
                        pbi = kt % 3

                        def f(e, kt=kt, pbi=pbi):
                            ins = None
                            for c2 in range(2):
                                ins = e.matmul(pbank[4 + c2][:, :], lhsT=rv[:, kt, c2 * 128:(c2 + 1) * 128], rhs=ST[pbi],
                                               start=(kt == 0), stop=(kt == NT - 1))
                            return ins
                        P.op("pe", f, r=[rvb, STb[pbi]], w=[PB[4], PB[5]])
                    issue_S(0)
                    issue_S(1)
                    for kt in range(NT):
                        issue_Y(kt)
                        if kt + 2 < NT:
                            issue_S(kt + 2)
                    for c2 in range(2):
                        P.op("act", lambda e, c2=c2: e.activation(out=sqr[:, c2, :], in_=pbank[4 + c2][:, :], func=AF.Square),
                             r=[PB[4 + c2]], w=[sqrb])
                    proj_group(3, lambda c: ones[:, :], lambda c: sqr[:, c, :], 2, pbank[3][:, :], [sqrb, cb])
                    P.op("act", lambda e: e.activation(out=lnv, in_=pbank[3][:, :], func=AF.Ln, scale=1.0 / 256.0, bias=epst[:, 0:1]),
                         r=[PB[3], cb], w=[lnvb])
                    P.op("act", lambda e: e.activation(out=rstd_r, in_=lnv, func=AF.Exp, scale=-0.5), r=[lnvb], w=[rstdb])
                    for c2 in range(2):
                        P.op("dve", lambda e, c2=c2: e.tensor_tensor(out=tmpy, in0=pbank[4 + c2][:, :], in1=rstd_r, op=ALU.mult),
                             r=[PB[4 + c2], rstdb], w=[tmpyb])
                        P.op("dve", lambda e, c2=c2, h=h, qg=qg: e.tensor_tensor(
                            out=retT[:, h * 2 + c2, qg * 512:(qg + 1) * 512], in0=tmpy, in1=sgl[:, c2, :], op=ALU.mult),
                            r=[tmpyb, sglb], w=[retTb])
            P.barrier()
            P.release(wr2b)
            phase_end("ret")
            arena.reset(m_ret)
            if layer == 0:
                dump("d_retT", lambda: retT2d, [retTb])

            m_mg = arena.mark()
            mergedT2d = arena.alloc(8 * T, BF16)
            mergedT = mergedT2d.rearrange("p (c t) -> p c t", c=8)
            mergedb = Buf("merged")
            wao = [arena.alloc(4 * 128, BF16).rearrange("p (c f) -> p c f", c=4) for _ in range(2)]
            wro = [arena.alloc(8 * 128, BF16).rearrange("p (c f) -> p c f", c=8) for _ in range(2)]
            wgt = [arena.alloc(8 * 2 * 128, BF16).rearrange("p (c w f) -> p c w f", c=8, w=2) for _ in range(2)]
            waob = [Buf("wao0"), Buf("wao1")]
            wrob = [Buf("wro0"), Buf("wro1")]
            wgtb = [Buf("wgt0"), Buf("wgt1")]
            sga = arena.alloc(512, F32)
            sgr = arena.alloc(512, F32)
            t1 = arena.alloc(512, F32)
            t2 = arena.alloc(512, F32)
            sgab, sgrb, t1b, t2b = Buf("sga"), Buf("sgr"), Buf("t1"), Buf("t2")
            wa_l = din["w_attn_out"][layer]
            wr_l = din["w_ret_out"][layer]
            for c in range(8):
                s = c % 2
                ld("pool", wao[s], bass.AP(wa_l.tensor, wa_l.offset + c * 128, [[D, 128], [128 * D, 4], [1, 128]]), waob[s])
                ld("pool", wro[s], bass.AP(wr_l.tensor, wr_l.offset + c * 128, [[D, 128], [128 * D, 8], [1, 128]]), wrob[s])
                ld_multi("pool", [(wgt[s][:, :, wh, :],
                                   bass.AP(w_in.tensor, w_in.offset + 8704 + wh * 1024 + c * 128, [[N_IN, 128], [128 * N_IN, 8], [1, 128]]))
                                  for wh in range(2)], wgtb[s])
                for tg in range(4):
                    b0 = 4 * (tg % 2)
                    tsl = slice(tg * 512, (tg + 1) * 512)
                    proj_group(b0, lambda k, s=s: wao[s][:, k, :], lambda k, tsl=tsl: attnT[:, k, tsl], 4, pbank[b0][:, :], [waob[s], attnTb])
                    proj_group(b0 + 1, lambda k, s=s: wro[s][:, k, :], lambda k, tsl=tsl: retT[:, k, tsl], 8, pbank[b0 + 1][:, :], [wrob[s], retTb])
                    proj_group(b0 + 2, lambda k, s=s: wgt[s][:, k, 0, :], lambda k, tsl=tsl: hT[:, k, tsl], 8, pbank[b0 + 2][:, :], [wgtb[s], hTb[tg]])
                    proj_group(b0 + 3, lambda k, s=s: wgt[s][:, k, 1, :], lambda k, tsl=tsl: hT[:, k, tsl], 8, pbank[b0 + 3][:, :], [wgtb[s], hTb[tg]])
                    P.op("act", lambda e, b0=b0: e.activation(out=sga, in_=pbank[b0 + 2][:, :], func=AF.Sigmoid), r=[PB[b0 + 2]], w=[sgab])
                    P.op("act", lambda e, b0=b0: e.activation(out=sgr, in_=pbank[b0 + 3][:, :], func=AF.Sigmoid), r=[PB[b0 + 3]], w=[sgrb])
                    P.op("dve", lambda e, b0=b0: e.tensor_tensor(out=t1, in0=pbank[b0][:, :], in1=sga, op=ALU.mult), r=[PB[b0], sgab], w=[t1b])
                    P.op("dve", lambda e, b0=b0: e.tensor_tensor(out=t2, in0=pbank[b0 + 1][:, :], in1=sgr, op=ALU.mult), r=[PB[b0 + 1], sgrb], w=[t2b])
                    P.op("dve", lambda e, c=c, tsl=tsl: e.tensor_tensor(out=mergedT[:, c, tsl], in0=t1, in1=t2, op=ALU.add), r=[t1b, t2b], w=[mergedb])
            P.barrier()
            P.release(waob + wrob + wgtb)
            phase_end("mrg")
            if layer == 0:
                dump("d_mergedT", lambda: mergedT2d, [mergedb])

            arena.reset(mix_mark)
            wo = [arena.alloc(8 * 512, BF16).rearrange("p (c f) -> p c f", c=8) for _ in range(2)]
            wob = [Buf("wo0"), Buf("wo1")]
            wo_l = din["w_out"][layer]
            for cg in range(2):
                ld("pool", wo[cg], bass.AP(wo_l.tensor, wo_l.offset + cg * 512, [[D, 128], [128 * D, 8], [1, 512]]), wob[cg])
            n = 0
            for cg in range(2):
                for t in range(NT):
                    pbi = n % 4
                    n += 1
                    proj_group(pbi, lambda k, t=t: mergedT[:, k, t * 128:(t + 1) * 128], lambda k, cg=cg: wo[cg][:, k, :], 8,
                               pbank[pbi][:, :], [mergedb, wob[cg]])
                    P.op("dve", lambda e, t=t, cg=cg, pbi=pbi: e.tensor_tensor(
                        out=x_sb[:, t, cg * 512:(cg + 1) * 512], in0=x_sb[:, t, cg * 512:(cg + 1) * 512], in1=pbank[pbi][:, :], op=ALU.add),
                        r=[PB[pbi], xb[t]], w=[xb[t]])
            P.barrier()
            P.release(wob)
            phase_end("out")
            arena.reset(mix_mark)
            if layer == 0:
                dump("d_x1", lambda: x_sb[:, :, :].rearrange("p t d -> p (t d)"), [xb[15]])

            moeb = Buf("moe_acc")
            ztile = arena.t32[:, (arena.nbytes - 4096) // 4: arena.nbytes // 4]
            ztb = Buf("ztile")
            P.op("dve", lambda e: e.memset(ztile, 0.0), w=[ztb])
            _zs = bass.AP(ztile.tensor, ztile.offset, [[ztile.ap[0][0], 128], [0, NT], [1, D]])
            P.dma("pool", lambda e, inc: inc(e.dma_start(out=moe_acc.rearrange("(t p) d -> p t d", p=128), in_=_zs)), r=[ztb], w=[moeb])
            afftm = arena.alloc(NT * 16, F32).rearrange("p (t e) -> p t e", t=NT)
            afftmb = Buf("afftm")
            mask_tm = arena.alloc(NT * 16, F32).rearrange("p (t e) -> p t e", t=NT)
            gm_tm = arena.alloc(NT * 16, F32).rearrange("p (t e) -> p t e", t=NT)
            pos_tm = arena.alloc(NT * 16, F32).rearrange("p (t e) -> p t e", t=NT)
            mask16 = arena.alloc(NT * 16, BF16).rearrange("p (t e) -> p t e", t=NT)
            masktb, gmtb, postb, m16b = Buf("mask_tm"), Buf("gm_tm"), Buf("pos_tm"), Buf("mask16")
            gm16_2d = arena.alloc(NT * 16 * 4, BF16)
            gm16 = gm16_2d.rearrange("p (t e two) -> p t e two", t=NT, e=16)
            gmlo = arena.alloc(NT * 16, F32).rearrange("p (t e) -> p t e", t=NT)
            gm16b = Buf("gm16")
            ffn_mark = arena.mark()
            h2tm2d = arena.alloc(NT * D, BF16)
            h2tm = h2tm2d.rearrange("p (t d) -> p t d", t=NT)
            h2tmb = [Buf("h2tm%d" % t) for t in range(NT)]
            hT2d = arena.alloc(8 * T, BF16)
            hT = hT2d.rearrange("p (c t) -> p c t", c=8)
            hTb = [Buf("h2T%d" % i) for i in range(4)]
            norm_phase(din["norm_ffn"][layer:layer + 1, :], hT, hTb, h_tm=h2tm, h_tmb=h2tmb)
            wrt = arena.alloc(8 * 16, BF16).rearrange("p (c e) -> p c e", c=8)
            wrtb = Buf("wrt")
            wr_l2 = din["w_router"][layer]
            ld("pool", wrt, wr_l2.rearrange("(c p) e -> p c e", p=128), wrtb)
            rmax = arena.alloc(NT, F32)
            rsum = arena.alloc(NT, F32)
            rmaxb, rsumb = Buf("rmax"), Buf("rsum")
            affT = arena.alloc(T, F32)
            work = arena.alloc(T, F32)
            maskT16 = arena.alloc(T, BF16)
            affTb, workb, maskT16b = Buf("affT"), Buf("work"), Buf("maskT16")
            m8 = arena.alloc(8, F32)
            m8b = Buf("m8")
            P.op("dve", lambda e: e.memset(rsum, 0.0), w=[rsumb])
            for t in range(NT):
                pbi = t % 2
                proj_group(pbi, lambda c, t=t: hT[:, c, t * 128:(t + 1) * 128], lambda c: wrt[:, c, :], 8, pbank[pbi][:, 0:16], [hTb[t // 4], wrtb])
                P.op("dve", lambda e, t=t, pbi=pbi: e.tensor_reduce(out=rmax[:, t:t + 1], in_=pbank[pbi][:, 0:16], axis=mybir.AxisListType.X,
                                                                   op=ALU.max, negate=True), r=[PB[pbi]], w=[rmaxb])
                P.op("act", lambda e, t=t, pbi=pbi: e.activation(out=afftm[:, t, :], in_=pbank[pbi][:, 0:16], func=AF.Exp,
                                                                 bias=rmax[:, t:t + 1], accum_out=rsum[:, t:t + 1]),
                     r=[PB[pbi], rmaxb], w=[afftmb, rsumb])
            P.op("dve", lambda e: e.reciprocal(out=rsum, in_=rsum), r=[rsumb], w=[rsumb])
            for t in range(NT):
                P.op("dve", lambda e, t=t: e.tensor_scalar(out=afftm[:, t, :], in0=afftm[:, t, :], scalar1=rsum[:, t:t + 1], scalar2=None, op0=ALU.mult),
                     r=[afftmb, rsumb], w=[afftmb])
            for tq in range(4):
                def f_tra(e, tq=tq):
                    ins = None
                    for k in range(4):
                        t = tq * 4 + k
                        ins = e.transpose(pbank[2 + tq % 2][0:16, k * 128:(k + 1) * 128], afftm[:, t, :], identf[:, :])
                    return ins
                P.op("pe", f_tra, r=[afftmb], w=[PB[2 + tq % 2]])
                P.op("act", lambda e, tq=tq: e.activation(out=affT[0:16, tq * 512:(tq + 1) * 512], in_=pbank[2 + tq % 2][0:16, :], func=AF.Copy),
                     r=[PB[2 + tq % 2]], w=[affTb])
            P.op("dve", lambda e: e.tensor_copy(out=work[0:16, :], in_=affT[0:16, :]), r=[affTb], w=[workb])
            for it8 in range(32):
                P.op("dve", lambda e: e.max(out=m8[0:16, :], in_=work[0:16, :]), r=[workb], w=[m8b])
                if it8 < 31:
                    P.op("dve", lambda e: e.match_replace(out=work[0:16, :], in_to_replace=m8[0:16, :], in_values=work[0:16, :], imm_value=-1.0),
                         r=[workb, m8b], w=[workb])
            P.op("dve", lambda e: e.tensor_scalar(out=maskT16[0:16, :], in0=affT[0:16, :], scalar1=m8[0:16, 7:8], scalar2=None, op0=ALU.is_ge),
                 r=[affTb, m8b], w=[maskT16b])

            def f_trm(e):
                ins = None
                for t in range(NT):
                    ins = e.transpose(pbank16[4][:, t * 16:(t + 1) * 16], maskT16[0:16, t * 128:(t + 1) * 128], ident[0:16, 0:16])
                return ins
            P.op("pe", f_trm, r=[maskT16b], w=[PB[4]])
            P.op("act", lambda e: e.activation(out=mask_tm[:, :, :], in_=pbank16[4][:, 0:NT * 16].rearrange("p (t e) -> p t e", t=NT), func=AF.Copy),
                 r=[PB[4]], w=[masktb])
            P.op("dve", lambda e: e.tensor_copy(out=mask16[:, :, :], in_=pbank16[4][:, 0:NT * 16].rearrange("p (t e) -> p t e", t=NT)),
                 r=[PB[4]], w=[m16b])
            P.op("dve", lambda e: e.tensor_tensor(out=gm_tm[:, :, :], in0=mask_tm[:, :, :], in1=afftm[:, :, :], op=ALU.mult), r=[masktb, afftmb], w=[gmtb])
            P.op("dve", lambda e: e.tensor_copy(out=gm16[:, :, :, 0], in_=gm_tm[:, :, :]), r=[gmtb], w=[gm16b])
            P.op("dve", lambda e: e.tensor_tensor(out=gmlo[:, :, :], in0=gm_tm[:, :, :], in1=gm16[:, :, :, 0], op=ALU.subtract), r=[gmtb, gm16b], w=[gm16b])
            P.op("dve", lambda e: e.tensor_copy(out=gm16[:, :, :, 1], in_=gmlo[:, :, :]), r=[gm16b], w=[gm16b])
            _tk = tokid[:, :]
            tok_bc = bass.AP(_tk.tensor, _tk.offset, [[_tk.ap[0][0], 128], [2, NT], [0, 16], [1, 2]])
            P.op("dve", lambda e: e.tensor_copy(out=gm16[:, :, :, 2:4], in_=tok_bc), r=[gm16b], w=[gm16b])
            for t in range(NT):
                pbi = t % 2

                def f_pos(e, t=t, pbi=pbi):
                    ins = None
                    for tp in range(t + 1):
                        lhs = ustrict[:, :] if tp == t else ones[:, :]
                        ins = e.matmul(pbank[pbi][:, 0:16], lhsT=lhs, rhs=mask16[:, tp, :], start=(tp == 0), stop=(tp == t))
                    return ins
                P.op("pe", f_pos, r=[m16b], w=[PB[pbi]])
                P.op("act", lambda e, t=t, pbi=pbi: e.activation(out=pos_tm[:, t, :], in_=pbank[pbi][:, 0:16], func=AF.Copy), r=[PB[pbi]], w=[postb])
            P.barrier()
            P.release([wrtb])
            phase_end("rt")
            if layer == 0:
                dump("d_afftm", lambda: afftm.rearrange("p t e -> p (t e)"), [afftmb])
                dump("d_mask", lambda: mask_tm.rearrange("p t e -> p (t e)"), [masktb])
                dump("d_pos", lambda: pos_tm.rearrange("p t e -> p (t e)"), [postb])
            arena.reset(ffn_mark)
            Sel = [arena.alloc(NT * 256, BF16).rearrange("p (t s) -> p t s", t=NT) for _ in range(2)]
            Selb = [Buf("Sel0"), Buf("Sel1")]
            mtile = [arena.alloc(D, F32) for _ in range(2)]
            mtb = [Buf("mt0"), Buf("mt1")]
            xinT = arena.alloc(8 * 256, BF16).rearrange("p (c s) -> p c s", c=8)
            xinTb = Buf("xinT")
            xin = [arena.alloc(2 * D, BF16).rearrange("p (k d) -> p k d", k=2) for _ in range(2)]
            xinb = [Buf("xin0"), Buf("xin1")]
            gsraw = [arena.alloc(8, F32) for _ in range(2)]
            gsrawb = [Buf("gsraw0"), Buf("gsraw1")]
            gs2 = [arena.alloc(2, F32) for _ in range(2)]
            gs2b = [Buf("gs0"), Buf("gs1")]
            idxf = [arena.alloc(2, F32) for _ in range(2)]
            idxi = [arena.alloc(2, F32).bitcast(mybir.dt.int32) for _ in range(2)]
            idxb = [Buf("idx0"), Buf("idx1")]
            actT = arena.alloc(16 * 256, BF16).rearrange("p (f s) -> p f s", f=16)
            actTb = Buf("actT")
            ysb = arena.alloc(2 * D, F32).rearrange("p (k d) -> p k d", k=2)
            ysbb = Buf("ysb")
            sa = [arena.alloc(256, F32) for _ in range(2)]
            sab = [Buf("sa0"), Buf("sa1")]
            NSLOT = 3
            wg = [arena.alloc(8 * 512, BF16).rearrange("p (c f) -> p c f", c=8) for _ in range(NSLOT)]
            wu = [arena.alloc(8 * 512, BF16).rearrange("p (c f) -> p c f", c=8) for _ in range(NSLOT)]
            wd = [arena.alloc(4 * D, BF16).rearrange("p (c d) -> p c d", c=4) for _ in range(NSLOT)]
            wgb = [Buf("wg%d" % i) for i in range(NSLOT)]
            wub = [Buf("wu%d" % i) for i in range(NSLOT)]
            wdb = [Buf("wd%d" % i) for i in range(NSLOT)]
            slab_ctr = [0]

            def gen_sel(ex, sl):
                for t in range(NT):
                    P.op("dve", lambda e, t=t: e.tensor_scalar(out=Sel[sl][:, t, :], in0=iota256[:, :], scalar1=pos_tm[:, t, ex:ex + 1],
                                                              scalar2=mask_tm[:, t, ex:ex + 1], op0=ALU.is_equal, op1=ALU.mult),
                         r=[postb, masktb], w=[Selb[sl]])
                    if t % 2 == 1:
                        yield
                S = Sel[sl]
                Sb = Selb[sl]
                def f_gs(e):
                    ins = None
                    for kk in range(2):
                        for t in range(NT):
                            ins = e.matmul(pbank[7][:, kk * 4:(kk + 1) * 4], lhsT=S[:, t, kk * 128:(kk + 1) * 128], rhs=gm16[:, t, ex, :],
                                           start=(t == 0), stop=(t == NT - 1))
                    return ins
                P.op("pe", f_gs, r=[Sb, gm16b], w=[PB[7]])
                P.op("act", lambda e: e.activation(out=gsraw[sl], in_=pbank[7][:, 0:8], func=AF.Copy), r=[PB[7]], w=[gsrawb[sl]])
                raw = gsraw[sl].rearrange("p (k f) -> p k f", k=2)
                P.op("dve", lambda e: e.tensor_tensor(out=gs2[sl], in0=raw[:, :, 0], in1=raw[:, :, 1], op=ALU.add), r=[gsrawb[sl]], w=[gs2b[sl]])
                P.op("dve", lambda e: e.scalar_tensor_tensor(out=idxf[sl], in0=raw[:, :, 3], scalar=128.0, in1=raw[:, :, 2], op0=ALU.mult, op1=ALU.add),
                     r=[gsrawb[sl]], w=[idxb[sl]])
                P.op("dve", lambda e: e.tensor_copy(out=idxi[sl], in_=idxf[sl]), r=[idxb[sl]], w=[idxb[sl]])
                yield

            def issue_gather(sl):
                def f_gather(e, inc):
                    for kk in range(2):
                        inc(e.indirect_dma_start(out=xin[sl][:, kk, :], out_offset=None, in_=h2_scr,
                                                 in_offset=bass.IndirectOffsetOnAxis(ap=idxi[sl][:, kk:kk + 1], axis=0)))
                P.dma("pool", f_gather, r=[idxb[sl]], w=[xinb[sl]], nd=2)

            LOOKAHEAD = NSLOT - 1

            def issue_load(n):
                if n >= 64:
                    return
                ex_, sl4_ = n // 4, n % 4
                s = n % NSLOT
                f0 = sl4_ * 512
                wg_l = din["w_gate"][layer, ex_]
                wu_l = din["w_up"][layer, ex_]
                wd_l = din["w_down"][layer, ex_]
                ld("pool", wg[s], bass.AP(wg_l.tensor, wg_l.offset + f0, [[2048, 128], [128 * 2048, 8], [1, 512]]), wgb[s])
                ld("pool", wu[s], bass.AP(wu_l.tensor, wu_l.offset + f0, [[2048, 128], [128 * 2048, 8], [1, 512]]), wub[s])
                ld("pool", wd[s], bass.AP(wd_l.tensor, wd_l.offset + f0 * D, [[D, 128], [128 * D, 4], [1, D]]), wdb[s])

            def gen_main(ex, sl):
                for kk in range(2):
                    pbi = 6 + (kk + 1) % 2

                    def f_xt(e, kk=kk, pbi=pbi):
                        ins = None
                        for c in range(8):
                            ins = e.transpose(pbank16[pbi][:, c * 128:(c + 1) * 128], xin[sl][:, kk, c * 128:(c + 1) * 128], ident[:, :])
                        return ins
                    P.op("pe", f_xt, r=[xinb[sl]], w=[PB[pbi]])
                    evac(xinT[:, :, kk * 128:(kk + 1) * 128], pbank16[pbi][:, 0:1024].rearrange("p (c f) -> p c f", c=8), [PB[pbi]], [xinTb])
                yield False
                for sl4 in range(4):
                    n_slab = ex * 4 + sl4
                    s = n_slab % NSLOT
                    issue_load(n_slab + LOOKAHEAD)
                    if sl4 == 3:
                        yield "last_load"
                    for fc in range(4):
                        fch = sl4 * 4 + fc
                        pbi = 4 + fch % 2

                        def f_ab(e, s=s, fc=fc, pbi=pbi):
                            ins = None
                            for c in range(8):
                                ins = e.matmul(pbank[pbi][:, 0:256], lhsT=wg[s][:, c, fc * 128:(fc + 1) * 128], rhs=xinT[:, c, :],
                                               start=(c == 0), stop=(c == 7))
                            for c in range(8):
                                ins = e.matmul(pbank[pbi][:, 256:512], lhsT=wu[s][:, c, fc * 128:(fc + 1) * 128], rhs=xinT[:, c, :],
                                               start=(c == 0), stop=(c == 7))
                            return ins
                        P.op("pe", f_ab, r=[wgb[s], wub[s], xinTb], w=[PB[pbi]])
                        si = fch % 2
                        P.op("act", lambda e, si=si, pbi=pbi: e.activation(out=sa[si], in_=pbank[pbi][:, 0:256], func=AF.Silu), r=[PB[pbi]], w=[sab[si]])
                        P.op("dve", lambda e, si=si, pbi=pbi, fch=fch: e.tensor_tensor(out=actT[:, fch, :], in0=sa[si], in1=pbank[pbi][:, 256:512], op=ALU.mult),
                             r=[sab[si], PB[pbi]], w=[actTb])
                        yield True
                    def f_dn(e, s=s, sl4=sl4):
                        ins = None
                        for k in range(2):
                            for dg in range(2):
                                for fc in range(4):
                                    fch = sl4 * 4 + fc
                                    ins = e.matmul(pbank[k * 2 + dg][:, :], lhsT=actT[:, fch, k * 128:(k + 1) * 128], rhs=wd[s][:, fc, dg * 512:(dg + 1) * 512],
                                                   start=(fch == 0), stop=(fch == 15))
                        return ins
                    P.op("pe", f_dn, r=[actTb, wdb[s]], w=[PB[0], PB[1], PB[2], PB[3]])
                    yield True
                n = 0
                for k in range(2):
                    for dg in range(2):
                        bi = k * 2 + dg
                        if n % 2 == 0:
                            P.op("act", lambda e, k=k, dg=dg, bi=bi: e.activation(out=ysb[:, k, dg * 512:(dg + 1) * 512], in_=pbank[bi][:, :],
                                                                                func=AF.Copy, scale=gs2[sl][:, k:k + 1]), r=[PB[bi], gs2b[sl]], w=[ysbb])
                        else:
                            P.op("dve", lambda e, k=k, dg=dg, bi=bi: e.tensor_scalar(out=ysb[:, k, dg * 512:(dg + 1) * 512], in0=pbank[bi][:, :],
                                                                                   scalar1=gs2[sl][:, k:k + 1], scalar2=None, op0=ALU.mult),
                                 r=[PB[bi], gs2b[sl]], w=[ysbb])
                        n += 1
                def f_scat(e, inc):
                    for kk in range(2):
                        inc(e.indirect_dma_start(out=moe_acc, out_offset=bass.IndirectOffsetOnAxis(ap=idxi[sl][:, kk:kk + 1], axis=0),
                                                 in_=ysb[:, kk, :], in_offset=None, compute_op=ALU.add))
                P.dma("pool", f_scat, r=[ysbb, idxb[sl]], w=[moeb], nd=2)
                yield False

            for n0 in range(LOOKAHEAD):
                issue_load(n0)
            for _ in gen_sel(0, 0):
                pass
            issue_gather(0)
            for ex in range(16):
                g_sel = gen_sel(ex + 1, (ex + 1) % 2) if ex + 1 < 16 else iter(())
                for ok in gen_main(ex, ex % 2):
                    if ok == "last_load":
                        for _ in g_sel:
                            pass
                        if ex + 1 < 16:
                            issue_gather((ex + 1) % 2)
                    elif ok:
                        next(g_sel, None)
            for t in range(NT):
                mt = mtile[t % 2]
                P.dma("sp", lambda e, inc, t=t, mt=mt: inc(e.dma_start(out=mt, in_=moe_acc[t * 128:(t + 1) * 128, :])), r=[moeb], w=[mtb[t % 2]])
                P.op("dve", lambda e, t=t, mt=mt: e.tensor_tensor(out=x_sb[:, t, :], in0=x_sb[:, t, :], in1=mt, op=ALU.add), r=[mtb[t % 2], xb[t]], w=[xb[t]])
            P.barrier()
            P.release(wgb + wub + wdb)
            phase_end("exp")
            arena.reset(mix_mark)
            if layer == 0:
                dump("d_x2", lambda: x_sb[:, :, :].rearrange("p t d -> p (t d)"), [xb[15]])

        norm_phase(din["norm_final"][0:1, :], None, None, final_out=out_d)
        P.barrier()
        P.disabled = False
        P.barrier()
        print("arena peak bytes/partition:", arena.peak, "dma sems:", len(P.dcount),
              "instr:", {e: len(P.q[e]) for e in ENGS})
        P.emit()
    return nc


_CACHE = {}


def _get_program():
    if "nc" not in _CACHE:
        _CACHE["nc"] = build_program()
    return _CACHE["nc"]


def make_in_maps(inputs, nb=8):
    c = _host_consts()
    shared = {
        "w_in": np.ascontiguousarray(inputs["w_in"], dtype=np.float32),
        "w_attn_out": np.ascontiguousarray(inputs["w_attn_out"], dtype=np.float32),
        "w_ret_out": np.ascontiguousarray(inputs["w_ret_out"], dtype=np.float32),
        "w_out": np.ascontiguousarray(inputs["w_out"], dtype=np.float32),
        "rdl": np.ascontiguousarray(inputs["ret_decay_logit"], dtype=np.float32).reshape(1, DEPTH * 8),
        "norm_mix": np.ascontiguousarray(inputs["norm_mix"], dtype=np.float32),
        "norm_ffn": np.ascontiguousarray(inputs["norm_ffn"], dtype=np.float32),
        "norm_final": np.ascontiguousarray(inputs["norm_final"], dtype=np.float32).reshape(1, D),
        "w_router": np.ascontiguousarray(inputs["w_router"], dtype=np.float32),
        "w_gate": np.ascontiguousarray(inputs["w_gate"], dtype=np.float32),
        "w_up": np.ascontiguousarray(inputs["w_up"], dtype=np.float32),
        "w_down": np.ascontiguousarray(inputs["w_down"], dtype=np.float32),
    }
    for k, v in c.items():
        shared["c_" + k] = v
    x = np.asarray(inputs["x"], dtype=np.float32)
    maps = []
    for b in range(nb):
        m = dict(shared)
        m["x"] = np.ascontiguousarray(x[b])
        maps.append(m)
    return maps


def kernel(**inputs):
    nc = _get_program()
    in_maps = make_in_maps(inputs)
    res = run_bass_kernel_spmd(nc, in_maps, core_ids=list(range(8)))
    return np.stack([np.asarray(r["out"], dtype=np.float32) for r in res.results], axis=0)
```
